# Optimizing a Trainium2 kernel written in Bass

```python
import math
import jax, jax.numpy as jnp
from jax import lax
import numpy as np

D_MODEL = 2048
BATCH = 8
SEQ = 4096
DEPTH = 4

CHUNK = 64
Q_BLOCK = 128
HEAD_DIM = 128
EPS = 1e-6
NEG = -1e30

A_HEADS = D_MODEL // (2 * HEAD_DIM)
A_LEFT_CHUNKS = 8
A_BAND = (A_LEFT_CHUNKS + 1) * CHUNK
A_REL_CLIP = 128
B_HEADS = D_MODEL // (4 * HEAD_DIM)
B_V_DIM = 2 * HEAD_DIM
C_HEADS = D_MODEL // HEAD_DIM
C_LATENT = D_MODEL // 8
IDX_HEADS = 8
IDX_DIM = 64
IDX_TOPK = 256
T5_BUCKETS = 32
T5_MAX_DIST = 128
T5_HEADS = B_HEADS + C_HEADS
D_FF = ((8 * D_MODEL + 3 * 256 - 1) // (3 * 256)) * 256

A_W = A_HEADS * HEAD_DIM
B_QK_W = B_HEADS * 2 * HEAD_DIM
B_V_W = B_HEADS * B_V_DIM
EVEN_PROJ = 3 * A_W + 2 * B_QK_W + B_V_W
EVEN_MIX = A_W + B_V_W
C_Q_W = C_HEADS * HEAD_DIM
ODD_PROJ = C_Q_W + C_LATENT + IDX_HEADS * IDX_DIM + IDX_DIM + IDX_HEADS
N_EVEN = (DEPTH + 1) // 2
N_ODD = DEPTH // 2

kernel_name = 'hybrid_chunk_causal_encoder'


def rmsnorm(x, g):
    xf = x.astype(jnp.float32)
    y = xf * lax.rsqrt(jnp.mean(xf * xf, axis=-1, keepdims=True) + EPS)
    return (y * g.astype(jnp.float32)).astype(x.dtype)


def t5_bucket(rel):
    nb = T5_BUCKETS // 2
    max_exact = nb // 2
    n = jnp.abs(rel)
    large = max_exact + (jnp.log(jnp.maximum(n, 1).astype(jnp.float32) / max_exact)
                         / math.log(T5_MAX_DIST / max_exact) * (nb - max_exact)).astype(jnp.int32)
    large = jnp.minimum(large, nb - 1)
    return jnp.where(rel > 0, nb, 0) + jnp.where(n < max_exact, n, large)


def chunk_end(pos):
    return (pos // CHUNK + 1) * CHUNK


def to_blocks(a, blk):
    b, s = a.shape[:2]
    return a.reshape(b, s // blk, blk, *a.shape[2:]).swapaxes(0, 1)


def from_blocks(a):
    nb, b, blk = a.shape[:3]
    return a.swapaxes(0, 1).reshape(b, nb * blk, *a.shape[3:])


def chunked_relpos_attention(q, k, v, rel_table):
    pad = A_LEFT_CHUNKS * CHUNK
    kp = jnp.pad(k, ((0, 0), (pad, 0), (0, 0), (0, 0)))
    vp = jnp.pad(v, ((0, 0), (pad, 0), (0, 0), (0, 0)))
    qi = jnp.arange(CHUNK)
    kj = jnp.arange(A_BAND)
    dist = jnp.clip(qi[:, None] + pad - kj[None, :], -A_REL_CLIP, A_REL_CLIP) + A_REL_CLIP
    bias = rel_table[dist].transpose(2, 0, 1).astype(jnp.float32)
    scale = HEAD_DIM ** -0.5

    def one_chunk(args):
        start, qc = args
        kb = lax.dynamic_slice_in_dim(kp, start, A_BAND, axis=1)
        vb = lax.dynamic_slice_in_dim(vp, start, A_BAND, axis=1)
        s = jnp.einsum('bqhd,bkhd->bhqk', qc, kb, preferred_element_type=jnp.float32) * scale + bias
        valid = (start - pad + kj) >= 0
        s = jnp.where(valid, s, NEG)
        p = jax.nn.softmax(s, axis=-1).astype(vb.dtype)
        return jnp.einsum('bhqk,bkhd->bqhd', p, vb)

    starts = jnp.arange(q.shape[1] // CHUNK) * CHUNK
    return from_blocks(lax.map(one_chunk, (starts, to_blocks(q, CHUNK))))


def diff_attention(q, k, v, t5_b, lam_vecs, subln_g, lam_init):
    s_len = q.shape[1]
    lv = lam_vecs.astype(jnp.float32)
    lam = jnp.exp(jnp.sum(lv[0] * lv[1])) - jnp.exp(jnp.sum(lv[2] * lv[3])) + lam_init
    kpos = jnp.arange(s_len)
    scale = HEAD_DIM ** -0.5

    def one_block(args):
        start, qb = args
        qpos = start + jnp.arange(Q_BLOCK)
        s = jnp.einsum('bqhmd,bkhmd->bhmqk', qb, k, preferred_element_type=jnp.float32) * scale
        bias = t5_b[t5_bucket(kpos[None, :] - qpos[:, None])].transpose(2, 0, 1).astype(jnp.float32)
        mask = kpos[None, :] < chunk_end(qpos)[:, None]
        s = jnp.where(mask, s + bias[:, None], NEG)
        p = jax.nn.softmax(s, axis=-1)
        a = (p[:, :, 0] - lam * p[:, :, 1]).astype(v.dtype)
        o = jnp.einsum('bhqk,bkhe->bqhe', a, v)
        return rmsnorm(o, subln_g) * (1.0 - lam_init)

    starts = jnp.arange(s_len // Q_BLOCK) * Q_BLOCK
    return from_blocks(lax.map(one_block, (starts, to_blocks(q, Q_BLOCK))))


def indexed_sparse_attention(q, c_lat, q_idx, k_idx, w_idx, t5_c, w_uk, w_uv):
    s_len = q.shape[1]
    k_sel = min(IDX_TOPK, s_len // 4)
    kpos = jnp.arange(s_len)
    scale = HEAD_DIM ** -0.5

    def one_block(args):
        start, qb, qib, wb = args
        qpos = start + jnp.arange(Q_BLOCK)
        end = chunk_end(qpos)
        rel = jax.nn.relu(jnp.einsum('bqhd,bsd->bqhs', qib, k_idx, preferred_element_type=jnp.float32))
        score = jnp.einsum('bqhs,bqh->bqs', rel, wb.astype(jnp.float32))
        score = jnp.where(kpos[None, None, :] < end[None, :, None], score, NEG)
        _, sel = lax.top_k(score, k_sel)
        valid = sel < end[None, :, None]
        c_sel = jax.vmap(lambda c, i: c[i])(c_lat, sel)
        q_lat = jnp.einsum('bqhd,hcd->bqhc', qb, w_uk)
        s = jnp.einsum('bqhc,bqkc->bhqk', q_lat, c_sel, preferred_element_type=jnp.float32) * scale
        bias = t5_c[t5_bucket(sel - qpos[None, :, None])].transpose(0, 3, 1, 2).astype(jnp.float32)
        s = jnp.where(valid[:, None], s + bias, NEG)
        p = jax.nn.softmax(s, axis=-1).astype(c_sel.dtype)
        o_lat = jnp.einsum('bhqk,bqkc->bqhc', p, c_sel)
        return jnp.einsum('bqhc,hcd->bqhd', o_lat, w_uv)

    starts = jnp.arange(s_len // Q_BLOCK) * Q_BLOCK
    out = lax.map(one_block, (starts, to_blocks(q, Q_BLOCK), to_blocks(q_idx, Q_BLOCK),
                              to_blocks(w_idx, Q_BLOCK)))
    return from_blocks(out)


def setup_inputs(seed: int = 0) -> dict:
    key = jax.random.key(seed)
    ks = jax.random.split(key, 20)

    def nrm(k, shape, scale):
        return jax.random.normal(k, shape, jnp.float32) * scale

    return {
        'x': nrm(ks[0], (BATCH, SEQ, D_MODEL), 1.0),
        'attn_norm': 1.0 + nrm(ks[1], (DEPTH, D_MODEL), 0.01),
        'ffn_norm': 1.0 + nrm(ks[2], (DEPTH, D_MODEL), 0.01),
        'final_norm': 1.0 + nrm(ks[3], (D_MODEL,), 0.01),
        't5_table': nrm(ks[4], (T5_BUCKETS, T5_HEADS), 0.5),
        'even_w_in': nrm(ks[5], (N_EVEN, D_MODEL, EVEN_PROJ), D_MODEL ** -0.5),
        'even_w_out': nrm(ks[6], (N_EVEN, EVEN_MIX, D_MODEL), EVEN_MIX ** -0.5),
        'a_rel_bias': nrm(ks[7], (N_EVEN, 2 * A_REL_CLIP + 1, A_HEADS), 0.5),
        'b_lambda': nrm(ks[8], (N_EVEN, 4, HEAD_DIM), 0.1),
        'b_subln': 1.0 + nrm(ks[9], (N_EVEN, B_V_DIM), 0.01),
        'odd_w_in': nrm(ks[10], (N_ODD, D_MODEL, ODD_PROJ), D_MODEL ** -0.5),
        'c_kv_norm': 1.0 + nrm(ks[11], (N_ODD, C_LATENT), 0.01),
        'c_w_uk': nrm(ks[12], (N_ODD, C_HEADS, C_LATENT, HEAD_DIM), C_LATENT ** -0.5),
        'c_w_uv': nrm(ks[13], (N_ODD, C_HEADS, C_LATENT, HEAD_DIM), C_LATENT ** -0.5),
        'odd_w_out': nrm(ks[14], (N_ODD, C_Q_W, D_MODEL), C_Q_W ** -0.5),
        'w_gate': nrm(ks[15], (DEPTH, D_MODEL, D_FF), D_MODEL ** -0.5),
        'w_up': nrm(ks[16], (DEPTH, D_MODEL, D_FF), D_MODEL ** -0.5),
        'w_down': nrm(ks[17], (DEPTH, D_FF, D_MODEL), D_FF ** -0.5),
    }


def reference(x, attn_norm, ffn_norm, final_norm, t5_table, even_w_in, even_w_out, a_rel_bias,
              b_lambda, b_subln, odd_w_in, c_kv_norm, c_w_uk, c_w_uv, odd_w_out, w_gate, w_up, w_down):
    b, s, _ = x.shape
    t5_b = t5_table[:, :B_HEADS]
    t5_c = t5_table[:, B_HEADS:]
    for layer in range(DEPTH):
        h = rmsnorm(x, attn_norm[layer])
        if layer % 2 == 0:
            e = layer // 2
            p = h @ even_w_in[e]
            qa, ka, va, qb, kb, vb = jnp.split(
                p, [A_W, 2 * A_W, 3 * A_W, 3 * A_W + B_QK_W, 3 * A_W + 2 * B_QK_W], axis=-1)
            oa = chunked_relpos_attention(qa.reshape(b, s, A_HEADS, HEAD_DIM),
                                          ka.reshape(b, s, A_HEADS, HEAD_DIM),
                                          va.reshape(b, s, A_HEADS, HEAD_DIM), a_rel_bias[e])
            lam_init = 0.8 - 0.6 * math.exp(-0.3 * layer)
            ob = diff_attention(qb.reshape(b, s, B_HEADS, 2, HEAD_DIM),
                                kb.reshape(b, s, B_HEADS, 2, HEAD_DIM),
                                vb.reshape(b, s, B_HEADS, B_V_DIM),
                                t5_b, b_lambda[e], b_subln[e], lam_init)
            mix = jnp.concatenate([oa.reshape(b, s, A_W), ob.reshape(b, s, B_V_W)], axis=-1) @ even_w_out[e]
        else:
            o = layer // 2
            p = h @ odd_w_in[o]
            o1 = C_Q_W
            o2 = o1 + C_LATENT
            o3 = o2 + IDX_HEADS * IDX_DIM
            o4 = o3 + IDX_DIM
            qc, cl, qi, ki, wi = jnp.split(p, [o1, o2, o3, o4], axis=-1)
            oc = indexed_sparse_attention(qc.reshape(b, s, C_HEADS, HEAD_DIM),
                                          rmsnorm(cl, c_kv_norm[o]),
                                          qi.reshape(b, s, IDX_HEADS, IDX_DIM), ki, wi,
                                          t5_c, c_w_uk[o], c_w_uv[o])
            mix = oc.reshape(b, s, C_Q_W) @ odd_w_out[o]
        x = x + mix
        h = rmsnorm(x, ffn_norm[layer])
        x = x + (jax.nn.silu(h @ w_gate[layer]) * (h @ w_up[layer])) @ w_down[layer]
    return rmsnorm(x, final_norm)
```

```python
import math
from contextlib import ExitStack

import numpy as np
import concourse.bass as bass
import concourse.mybir as mybir
from concourse.bass_utils import run_bass_kernel_spmd

F32 = mybir.dt.float32
BF16 = mybir.dt.bfloat16
AF = mybir.ActivationFunctionType
ALU = mybir.AluOpType
AX = mybir.AxisListType

D = 2048
S = 4096
DEPTH = 4
NT = S // 512
DFF = 5632
NJ = DFF // 128
EPS = 1e-6
NEG = -30000.0
SCALE = 128 ** -0.5
ODD_PROJ = 2888
ARENA_WORDS = 51200


class Res:
    __slots__ = ("name", "w", "r")

    def __init__(self, name=""):
        self.name = name
        self.w = None
        self.r = {}


class Sched:
    ENGS = ("pe", "act", "dve", "pool", "sp")
    NDMA = 8

    def __init__(self, nc, stack):
        self.nc = nc
        self.q = {e: [] for e in self.ENGS}
        self.sem = {e: stack.enter_context(nc.semaphore("s_" + e)) for e in self.ENGS}
        self.cnt = {e: 0 for e in self.ENGS}
        self.known = {e: {} for e in self.ENGS}
        self.pending = {e: {} for e in self.ENGS}
        self.dsem = {}
        self.dcnt = {}
        self.drr = {}
        for qn in ("sp", "pool", "act"):
            self.dsem[qn] = [stack.enter_context(nc.semaphore("d_%s%d" % (qn, i))) for i in range(self.NDMA)]
            self.drr[qn] = 0
            for s in self.dsem[qn]:
                self.dcnt[id(s)] = 0
        self.nops = 0

    def _collect(self, eng, reads, writes, acc):
        waits = dict(self.pending[eng])
        self.pending[eng] = {}
        kn = self.known[eng]
        own = self.sem[eng] if eng in self.sem else None

        def need(ev):
            if ev is None:
                return
            s, v = ev
            k = id(s)
            if kn.get(k, 0) >= v:
                return
            if k not in waits or waits[k][1] < v:
                waits[k] = (s, v)

        for r in reads:
            need(r.w)
        for w in writes:
            if not (acc and w.w is not None and w.w[0] is own):
                need(w.w)
            for ev in w.r.values():
                need(ev)
        return waits

    def _commit(self, eng, reads, writes, ev, waits):
        kn = self.known[eng]
        for k, (s, v) in waits.items():
            if kn.get(k, 0) < v:
                kn[k] = v
        k = id(ev[0])
        for r in reads:
            r.r[k] = ev
        for w in writes:
            w.w = ev
            w.r = {}

    def op(self, eng, fn, reads=(), writes=(), acc=False):
        waits = self._collect(eng, reads, writes, acc)
        self.cnt[eng] += 1
        ev = (self.sem[eng], self.cnt[eng])
        self._commit(eng, reads, writes, ev, waits)
        self.q[eng].append((list(waits.values()), fn, (self.sem[eng], 1)))
        self.nops += 1
        return ev

    def dma(self, qn, out_ap, in_ap, reads=(), writes=()):
        eng = qn
        i = self.drr[qn] % self.NDMA
        self.drr[qn] += 1
        s = self.dsem[qn][i]
        waits = self._collect(eng, reads, writes, False)
        prev = self.dcnt[id(s)]
        if prev > 0 and self.known[eng].get(id(s), 0) < prev:
            waits[id(s)] = (s, prev)
        self.dcnt[id(s)] = prev + 16
        ev = (s, prev + 16)
        self._commit(eng, reads, writes, ev, waits)

        def fn(e, out_ap=out_ap, in_ap=in_ap):
            return e.dma_start(out=out_ap, in_=in_ap)

        self.q[eng].append((list(waits.values()), fn, (s, 16)))
        self.nops += 1
        return ev

    def barrier(self):
        evs = []
        for e in self.ENGS:
            if self.cnt[e] > 0:
                evs.append((self.sem[e], self.cnt[e]))
        for qn in self.dsem:
            for s in self.dsem[qn]:
                if self.dcnt[id(s)] > 0:
                    evs.append((s, self.dcnt[id(s)]))
        for e in self.ENGS:
            kn = self.known[e]
            for (s, v) in evs:
                if kn.get(id(s), 0) < v:
                    p = self.pending[e]
                    if id(s) not in p or p[id(s)][1] < v:
                        p[id(s)] = (s, v)

    def finish(self, eng="sp"):
        self.barrier()
        waits = self.pending[eng]
        self.pending[eng] = {}
        self.q[eng].append((list(waits.values()), None, None))

    def replay(self, eng, e):
        for (waits, fn, inc) in self.q[eng]:
            for (s, v) in waits:
                e.wait_ge(s, v)
            if fn is not None:
                ins = fn(e)
                ins.then_inc(inc[0], inc[1])


class Arena:
    def __init__(self, big):
        self.big = big
        self.off = 0

    def reset(self):
        self.off = 0

    def f32(self, n):
        assert self.off + n <= ARENA_WORDS, "arena overflow %d" % (self.off + n)
        ap = self.big[:, self.off:self.off + n]
        self.off += n
        return ap

    def bf16(self, n):
        w = (n + 1) // 2
        ap = self.f32(w).bitcast(BF16)
        return ap[:, 0:n]


class Ctx:
    pass


def v3(ap, c):
    return ap.rearrange("p (c t) -> p c t", c=c)


def mm_group(Sc, out_ap, terms, reads, out_res):
    terms = list(terms)

    def fn(e):
        n = len(terms)
        ins = None
        for i, (l, r) in enumerate(terms):
            ins = e.matmul(out_ap, l, r, start=(i == 0), stop=(i == n - 1))
        return ins

    return Sc.op("pe", fn, reads=reads, writes=[out_res])


def rmsnorm_fm(Sc, cx, xt, xt_res, hT, hT_res, g, g_res, nchunk, dim, out_f32=None):
    pb, pr = cx.bank()
    for c in range(nchunk):
        sq, sqr = cx.sq[c % len(cx.sq)]
        Sc.op("act", lambda e, o=sq, i=xt[:, c, :]: e.activation(out=o, in_=i, func=AF.Square),
              reads=[xt_res], writes=[sqr])
        Sc.op("pe", lambda e, o=pb, r=sq, c=c: e.matmul(o, cx.ones_f[:, :], r, start=(c == 0), stop=(c == nchunk - 1)),
              reads=[sqr, cx.const_res], writes=[pr], acc=(c > 0))
    rstd, rr = cx.rstd
    Sc.op("act", lambda e: e.activation(out=rstd, in_=pb, func=AF.Sqrt, bias=EPS, scale=1.0 / dim),
          reads=[pr], writes=[rr])
    Sc.op("dve", lambda e: e.reciprocal(out=rstd, in_=rstd), reads=[rr], writes=[rr])
    for c in range(nchunk):
        Sc.op("dve", lambda e, c=c: e.scalar_tensor_tensor(out=hT[:, c, :], in0=xt[:, c, :], scalar=g[:, c:c + 1],
                                                            in1=rstd, op0=ALU.mult, op1=ALU.mult),
              reads=[xt_res, g_res, rr], writes=[hT_res])


class WStream:
    def __init__(self, Sc, bufs, blocks, r_w, pf, hold=1):
        self.Sc, self.bufs, self.blocks, self.r_w, self.pf = Sc, bufs, blocks, r_w, pf
        self.next = 0
        assert pf <= len(bufs) - hold

    def ensure(self, k):
        while self.next < len(self.blocks) and self.next <= k:
            buf, res = self.bufs[self.next % len(self.bufs)]
            blk = self.blocks[self.next]
            n = blk.shape[1]
            self.Sc.dma("pool", buf[:, 0:n], blk, reads=[self.r_w], writes=[res])
            self.next += 1

    def get(self, i):
        self.ensure(i + self.pf)
        return self.bufs[i % len(self.bufs)]


def evac(Sc, cx, out_ap, in_ap, reads, writes, scale=None):
    cx.evrr += 1
    if cx.evrr % 2 == 0:
        if scale is None:
            Sc.op("act", lambda e: e.activation(out=out_ap, in_=in_ap, func=AF.Copy), reads=reads, writes=writes)
        else:
            Sc.op("act", lambda e: e.activation(out=out_ap, in_=in_ap, func=AF.Copy, scale=float(scale)),
                  reads=reads, writes=writes)
    else:
        if scale is None:
            Sc.op("dve", lambda e: e.tensor_copy(out=out_ap, in_=in_ap), reads=reads, writes=writes)
        else:
            Sc.op("dve", lambda e: e.tensor_scalar(out=out_ap, in0=in_ap, scalar1=float(scale), scalar2=None,
                                                   op0=ALU.mult), reads=reads, writes=writes)


T5_EDGES = [1, 2, 3, 4, 5, 6, 7, 8, 12, 16, 23, 32, 46, 64, 91]


def t5_bucket_static(rel):
    n = abs(rel)
    b = sum(1 for e in T5_EDGES if e <= n)
    return b + (16 if rel > 0 else 0)


def tile_view(t_ap, t):
    return v3(t_ap[t], 16)


def phase_inproj(Sc, cx, layer, T):
    even = (layer % 2 == 0)
    A = cx.arena
    A.reset()
    cx.alloc_consts()
    xt = [(v3(A.f32(16 * 512), 16), Res("xt")) for _ in range(2)]
    hTs = [(v3(A.bf16(16 * 512), 16), Res("hT")) for _ in range(2)]
    cx.sq = [(A.f32(512), Res("sq")) for _ in range(4)]
    cx.rstd = (A.f32(512), Res("rstd"))
    wbufs = [(A.bf16(16 * 512), Res("wb")) for _ in range(3)]
    stage = [(v3(A.bf16(4 * 512), 4), Res("stg")) for _ in range(3)]
    srr = [0]

    def get_stage():
        s_ = stage[srr[0] % 3]
        srr[0] += 1
        return s_

    g = A.f32(16)
    g_res = Res("g")
    Sc.dma("sp", g, T.attn_norm[layer], reads=[T.r_small], writes=[g_res])
    if not even:
        o = layer // 2
        gk = A.f32(2)
        gk_res = Res("gk")
        Sc.dma("sp", gk, T.c_kv_norm_fm[o], reads=[T.r_small], writes=[gk_res])
        gkb = A.f32(256)
        gkb_res = Res("gkb")
        Sc.dma("sp", gkb, T.c_kv_norm_bc[o], reads=[T.r_small], writes=[gkb_res])
        clf = (v3(A.f32(2 * 512), 2), Res("clf"))
        clo = (v3(A.bf16(2 * 512), 2), Res("clo"))
        cltm = (A.f32(256), Res("cltm"))
        cltm_o = (v3(A.bf16(4 * 256), 4), Res("cltmo"))
        ssq = (A.f32(4), Res("ssq"))
        junk = (A.f32(256), Res("junk"))
        wstage = (v3(A.f32(4 * 8), 4), Res("wst"))
    first = (layer == 0 or T.force_x_in)
    xsrc = T.x_in if first else T.xs
    xsrc_res = T.r_x_in if first else T.r_xs
    W = T.w_in_even[layer // 2] if even else T.w_in_odd[layer // 2]
    nblk = 12 if even else 6
    ws = WStream(Sc, wbufs, [W[nb] for t in range(NT) for nb in range(nblk)], T.r_w, 2)

    def load_x(t):
        Sc.dma("sp", xt[t % 2][0], tile_view(xsrc, t), reads=[xsrc_res], writes=[xt[t % 2][1]])

    load_x(0)
    ws.ensure(1)
    for t in range(NT):
        if t + 1 < NT:
            load_x(t + 1)
        x_ap, x_res = xt[t % 2]
        hT, hT_res = hTs[t % 2]
        rmsnorm_fm(Sc, cx, x_ap, x_res, hT, hT_res, g, g_res, 16, D)
        tok = slice(t * 512, (t + 1) * 512)
        for nb in range(nblk):
            wb, wres = ws.get(t * nblk + nb)
            wv = v3(wb, 16)
            if even:
                kind = "tm" if nb in (4, 5, 10, 11) else "fm"
            else:
                kind = "fm" if nb < 4 else ("odd4" if nb == 4 else "odd5")
            if kind == "fm":
                st, st_res = get_stage()
                scale = SCALE if (even and nb in (0, 1, 6, 7)) else None
                for sub in range(4):
                    pb, pr = cx.bank()
                    mm_group(Sc, pb, [(wv[:, c, sub * 128:(sub + 1) * 128], hT[:, c, :]) for c in range(16)],
                             [wres, hT_res], pr)
                    evac(Sc, cx, st[:, sub, :], pb, [pr], [st_res], scale)
                if even:
                    r0 = _even_fm_row(nb)
                    dst = T.qkT_even[r0:r0 + 512, tok]
                    dres = T.r_qkT
                else:
                    dst = T.qcT[nb * 512:(nb + 1) * 512, tok]
                    dres = T.r_qcT
                Sc.dma("pool", dst.rearrange("(s p) t -> p s t", p=128), st, reads=[st_res], writes=[dres])
            elif kind == "tm":
                st, st_res = get_stage()
                for sub in range(4):
                    pb, pr = cx.bank()
                    mm_group(Sc, pb, [(hT[:, c, sub * 128:(sub + 1) * 128], wv[:, c, :]) for c in range(16)],
                             [wres, hT_res], pr)
                    evac(Sc, cx, st[:, sub, :], pb, [pr], [st_res])
                if nb in (4, 5):
                    for hh in range(4):
                        h = (nb - 4) * 4 + hh
                        Sc.dma("pool", v3(T.v_a[h], 32)[:, t * 4:(t + 1) * 4, :], st[:, :, hh * 128:(hh + 1) * 128],
                               reads=[st_res], writes=[T.r_v])
                else:
                    for hh in range(2):
                        h = (nb - 10) * 2 + hh
                        Sc.dma("pool", v3(T.v_b[h], 32)[:, t * 4:(t + 1) * 4, :], st[:, :, hh * 256:(hh + 1) * 256],
                               reads=[st_res], writes=[T.r_v])
            elif kind == "odd4":
                clf_ap, clf_res = clf
                for cc in range(2):
                    pb, pr = cx.bank()
                    mm_group(Sc, pb, [(wv[:, c, cc * 128:(cc + 1) * 128], hT[:, c, :]) for c in range(16)],
                             [wres, hT_res], pr)
                    evac(Sc, cx, clf_ap[:, cc, :], pb, [pr], [clf_res])
                rmsnorm_fm(Sc, cx, clf_ap, clf_res, clo[0], clo[1], gk, gk_res, 2, 256)
                Sc.dma("pool", T.clatT[:, tok].rearrange("(s p) t -> p s t", p=128), clo[0], reads=[clo[1]],
                       writes=[T.r_clatT])
                for sub in range(4):
                    pb, pr = cx.bank()
                    mm_group(Sc, pb[:, 0:256],
                             [(hT[:, c, sub * 128:(sub + 1) * 128], wv[:, c, 0:256]) for c in range(16)],
                             [wres, hT_res], pr)
                    Sc.op("dve", lambda e, pb=pb: e.tensor_copy(out=cltm[0], in_=pb[:, 0:256]), reads=[pr],
                          writes=[cltm[1]])
                    Sc.op("act", lambda e: e.activation(out=junk[0], in_=cltm[0], func=AF.Square,
                                                        accum_out=ssq[0][:, 0:1]),
                          reads=[cltm[1]], writes=[junk[1], ssq[1]])
                    Sc.op("act", lambda e: e.activation(out=ssq[0][:, 1:2], in_=ssq[0][:, 0:1], func=AF.Sqrt, bias=EPS,
                                                        scale=1.0 / 256), reads=[ssq[1]], writes=[ssq[1]])
                    Sc.op("dve", lambda e: e.reciprocal(out=ssq[0][:, 2:3], in_=ssq[0][:, 1:2]), reads=[ssq[1]],
                          writes=[ssq[1]])
                    Sc.op("dve", lambda e, sub=sub: e.scalar_tensor_tensor(
                        out=cltm_o[0][:, sub, :], in0=cltm[0], scalar=ssq[0][:, 2:3], in1=gkb,
                        op0=ALU.mult, op1=ALU.mult), reads=[cltm[1], ssq[1], gkb_res], writes=[cltm_o[1]])
                Sc.dma("pool", v3(T.clat_tm, 32)[:, t * 4:(t + 1) * 4, :], cltm_o[0], reads=[cltm_o[1]],
                       writes=[T.r_clat_tm])
                st, st_res = get_stage()
                for sub in range(2):
                    pb, pr = cx.bank()
                    mm_group(Sc, pb,
                             [(wv[:, c, 256 + sub * 128:256 + (sub + 1) * 128], hT[:, c, :]) for c in range(16)],
                             [wres, hT_res], pr)
                    evac(Sc, cx, st[:, sub, :], pb, [pr], [st_res])
                Sc.dma("pool", T.qiT[0:256, tok].rearrange("(s p) t -> p s t", p=128), st[:, 0:2, :], reads=[st_res],
                       writes=[T.r_qiT])
            else:
                st, st_res = get_stage()
                for sub in range(2):
                    pb, pr = cx.bank()
                    mm_group(Sc, pb, [(wv[:, c, sub * 128:(sub + 1) * 128], hT[:, c, :]) for c in range(16)],
                             [wres, hT_res], pr)
                    evac(Sc, cx, st[:, sub, :], pb, [pr], [st_res])
                pb, pr = cx.bank()
                mm_group(Sc, pb[0:64, :], [(wv[:, c, 256:320], hT[:, c, :]) for c in range(16)], [wres, hT_res], pr)
                evac(Sc, cx, st[0:64, 2, :], pb[0:64, :], [pr], [st_res])
                Sc.dma("pool", T.qiT[256:512, tok].rearrange("(s p) t -> p s t", p=128), st[:, 0:2, :], reads=[st_res],
                       writes=[T.r_qiT])
                Sc.dma("pool", T.kiT[0:64, tok], st[0:64, 2, :], reads=[st_res], writes=[T.r_kiT])
                Sc.dma("pool", T.kiT[64:128, tok], st[0:64, 2, :], reads=[st_res], writes=[T.r_kiT])
                for sub in range(4):
                    pb, pr = cx.bank()
                    mm_group(Sc, pb[:, 0:8],
                             [(hT[:, c, sub * 128:(sub + 1) * 128], wv[:, c, 320:328]) for c in range(16)],
                             [wres, hT_res], pr)
                    Sc.op("dve", lambda e, pb=pb, sub=sub: e.tensor_copy(out=wstage[0][:, sub, :], in_=pb[:, 0:8]),
                          reads=[pr], writes=[wstage[1]])
                Sc.dma("pool", v3(T.wi_tm, 32)[:, t * 4:(t + 1) * 4, :], wstage[0], reads=[wstage[1]], writes=[T.r_wi])
    Sc.barrier()


def _even_fm_row(nb):
    return {0: 0, 1: 512, 2: 1024, 3: 1536, 6: 2048, 7: 2560, 8: 3072, 9: 3584}[nb]


def build_line_buffer(Sc, cx, tabT, tab_res, nh, segs, B_dram, B_res, fline):
    f_ap, f_res = fline
    for (a, b, src, rng) in segs:
        if rng:
            Sc.op("dve", lambda e, a=a, b=b, src=src: e.tensor_copy(out=f_ap[0:nh, a:b], in_=tabT[0:nh, src:src + (b - a)]),
                  reads=[tab_res], writes=[f_res])
        else:
            Sc.op("dve", lambda e, a=a, b=b, src=src: e.tensor_copy(
                out=f_ap[0:nh, a:b], in_=tabT[0:nh, src:src + 1].to_broadcast([nh, b - a])),
                  reads=[tab_res], writes=[f_res])
    pstep = f_ap.ap[0][0]
    src_ap = bass.AP(f_ap.tensor, f_ap.offset, [[pstep, nh], [0, 128], [1, 2048]])
    Sc.dma("pool", B_dram.rearrange("h (r m) -> h r m", m=2048), src_ap, reads=[f_res], writes=[B_res])


def build_bias_tile(Sc, cx, B_dram, B_res, h, base, dst_ap, dst_res, masked_blocks, tl):
    tl_ap, tl_res = tl
    src = bass.AP(B_dram.tensor, h * 128 * 2048 + base, [[2047, 128], [1, 512]])
    Sc.dma("pool", tl_ap, src, reads=[B_res], writes=[tl_res])
    for (a, b0, b1) in masked_blocks:
        Sc.op("pool", lambda e, a=a, b0=b0, b1=b1: e.memset(tl_ap[a * 64:(a + 1) * 64, b0 * 64:b1 * 64], NEG),
              reads=[], writes=[tl_res])
    Sc.op("act", lambda e: e.activation(out=dst_ap, in_=tl_ap, func=AF.Copy), reads=[tl_res], writes=[dst_res])


def mask_blocks(o, rule):
    out = []
    for a in range(2):
        masked = []
        for b in range(8):
            delta = o // 64 + a - b
            if rule == "A":
                ok = (-8 <= delta <= 0)
            elif rule == "B":
                ok = (delta <= 0)
            else:
                ok = True
            masked.append(not ok)
        b = 0
        while b < 8:
            if masked[b]:
                b1 = b
                while b1 < 8 and masked[b1]:
                    b1 += 1
                out.append((a, b, b1))
                b = b1
            else:
                b += 1
    return out


def attn_keytile(Sc, cx, s_terms, s_reads, bias_col, bias_res, pv, first, last):
    sb, sr = cx.sbanks[cx.srr % 2]
    cx.srr += 1
    mm_group(Sc, sb, s_terms, s_reads, sr)
    E, Er = cx.ebufs[cx.err % len(cx.ebufs)]
    cx.err += 1
    if bias_col is None:
        Sc.op("act", lambda e: e.activation(out=E, in_=sb, func=AF.Exp), reads=[sr], writes=[Er])
    else:
        Sc.op("act", lambda e: e.activation(out=E, in_=sb, func=AF.Exp, bias=bias_col), reads=[sr, bias_res],
              writes=[Er])
    for (l, lres, acc_ap, acc_res) in pv:
        Sc.op("pe", lambda e, l=l, acc_ap=acc_ap: e.matmul(acc_ap, l, E, start=first, stop=last),
              reads=[Er, lres, cx.const_res], writes=[acc_res], acc=(not first))


def phase_attnA(Sc, cx, layer, T):
    e_ = layer // 2
    A = cx.arena
    A.reset()
    cx.alloc_consts()
    biasA = (v3(A.bf16(64 * 512), 64), Res("biasA"))
    tl = [(A.f32(512), Res("tl")) for _ in range(2)]
    tabT = A.f32(260)
    tab_res = Res("tab")
    fline = (A.f32(2048), Res("fline"))
    QT = [(A.bf16(S), Res("QT")) for _ in range(2)]
    KT = [(A.bf16(S), Res("KT")) for _ in range(2)]
    V = [(v3(A.bf16(32 * 128), 32), Res("V")) for _ in range(2)]
    cx.ebufs = [(A.bf16(512), Res("E")) for _ in range(3)]
    cx.err = 0
    rden = [(A.f32(512), Res("rden")) for _ in range(2)]
    ost = [(A.bf16(512), Res("ost")) for _ in range(2)]
    cx.sbanks = cx.banks[0:2]
    cx.srr = 0
    Sc.dma("sp", tabT[0:8, 0:257], T.a_rel_T[e_], reads=[T.r_small], writes=[tab_res])
    segs = [(0, 384, 0, False), (384, 641, 0, True), (641, 2048, 256, False)]
    build_line_buffer(Sc, cx, tabT, tab_res, 8, segs, T.lineA, T.r_lineA, fline)
    offs = [-512, -384, -256, -128, 0, 128, 256, 384]
    n = 0
    for h in range(8):
        for oi, o in enumerate(offs):
            build_bias_tile(Sc, cx, T.lineA, T.r_lineA, h, 512 - o, biasA[0][:, h * 8 + oi, :], biasA[1],
                            mask_blocks(o, "A"), tl[n % 2])
            n += 1

    def load_head(h):
        Sc.dma("sp", QT[h % 2][0], T.qkT_even[h * 128:(h + 1) * 128, :], reads=[T.r_qkT], writes=[QT[h % 2][1]])
        Sc.dma("sp", KT[h % 2][0], T.qkT_even[1024 + h * 128:1024 + (h + 1) * 128, :], reads=[T.r_qkT],
               writes=[KT[h % 2][1]])
        Sc.dma("sp", V[h % 2][0], v3(T.v_a[h], 32), reads=[T.r_v], writes=[V[h % 2][1]])

    load_head(0)
    it = 0
    for h in range(8):
        if h + 1 < 8:
            load_head(h + 1)
        q_ap, q_res = QT[h % 2]
        k_ap, k_res = KT[h % 2]
        v_ap, v_res = V[h % 2]
        for qt in range(NT):
            kts = [kt for kt in range(4 * qt - 4, 4 * qt + 4) if kt >= 0]
            ob, obr = cx.banks[2 + (it % 2) * 2]
            db, dbr = cx.banks[3 + (it % 2) * 2]
            it += 1
            for idx, kt in enumerate(kts):
                oi = kt - (4 * qt - 4)
                s_terms = [(k_ap[:, kt * 128:(kt + 1) * 128], q_ap[:, qt * 512:(qt + 1) * 512]),
                           (cx.ident_b, biasA[0][:, h * 8 + oi, :])]
                pv = [(v_ap[:, kt, :], v_res, ob, obr), (cx.ones_b, cx.const_res, db, dbr)]
                attn_keytile(Sc, cx, s_terms, [k_res, q_res, biasA[1], cx.const_res], None, None, pv,
                             idx == 0, idx == len(kts) - 1)
            rd, rdr = rden[it % 2]
            os_, osr = ost[it % 2]
            Sc.op("dve", lambda e, rd=rd, db=db: e.reciprocal(out=rd, in_=db), reads=[dbr], writes=[rdr])
            Sc.op("dve", lambda e, os_=os_, ob=ob, rd=rd: e.tensor_tensor(out=os_, in0=ob, in1=rd, op=ALU.mult),
                  reads=[obr, rdr], writes=[osr])
            Sc.dma("pool", tile_view(T.mixT, qt)[:, h, :], os_, reads=[osr], writes=[T.r_mixT])
    Sc.barrier()


def t5_line_segs():
    segs = []
    m = 0
    while m < 2048:
        b = t5_bucket_static(1023 - m)
        m1 = m
        while m1 < 2048 and t5_bucket_static(1023 - m1) == b:
            m1 += 1
        segs.append((m, m1, b, False))
        m = m1
    return segs


def phase_setup_t5(Sc, cx, T):
    A = cx.arena
    A.reset()
    cx.alloc_consts()
    tabT = A.f32(32)
    tab_res = Res("tab")
    fline = (A.f32(2048), Res("fline"))
    Sc.dma("sp", tabT[0:20, 0:32], T.t5_T, reads=[T.r_small], writes=[tab_res])
    build_line_buffer(Sc, cx, tabT, tab_res, 20, t5_line_segs(), T.lineT5, T.r_lineT5, fline)
    Sc.barrier()


def phase_attnB(Sc, cx, layer, T):
    e_ = layer // 2
    lam_init = 0.8 - 0.6 * math.exp(-0.3 * layer)
    A = cx.arena
    A.reset()
    cx.alloc_consts()
    biasB = (v3(A.bf16(20 * 512), 20), Res("biasB"))
    tl = [(A.f32(512), Res("tl")) for _ in range(2)]
    QT = [(v3(A.bf16(2 * S), 2), Res("QT")) for _ in range(2)]
    KT = [(v3(A.bf16(2 * S), 2), Res("KT")) for _ in range(2)]
    V = [(v3(A.bf16(32 * 256), 32), Res("V")) for _ in range(2)]
    cx.ebufs = [(A.bf16(512), Res("E")) for _ in range(3)]
    cx.err = 0
    cx.sbanks = cx.banks[0:2]
    cx.srr = 0
    rden = (A.f32(512), Res("rden"))
    o1 = (v3(A.f32(2 * 512), 2), Res("o1"))
    oo = (v3(A.f32(2 * 512), 2), Res("oo"))
    sqb = (v3(A.bf16(2 * 512), 2), Res("sqb"))
    rstd = (A.f32(512), Res("rstdB"))
    ost = [(v3(A.bf16(2 * 512), 2), Res("ost")) for _ in range(2)]
    t5bc = A.f32(640)
    t5bc_res = Res("t5bc")
    Sc.dma("sp", t5bc, T.t5_bc, reads=[T.r_small], writes=[t5bc_res])
    lamb = A.f32(512)
    lam_res = Res("lam")
    Sc.dma("sp", lamb, T.b_lambda_bc[e_], reads=[T.r_small], writes=[lam_res])
    lsm = A.f32(8)
    junk = A.f32(128)
    junk_res = Res("junk")
    gs = A.f32(2)
    gs_res = Res("gs")
    Sc.dma("sp", gs, T.b_subln_fm[e_], reads=[T.r_small], writes=[gs_res])
    Sc.op("dve", lambda e: e.tensor_scalar(out=gs, in0=gs, scalar1=float(1.0 - lam_init), scalar2=None, op0=ALU.mult),
          reads=[gs_res], writes=[gs_res])
    for i in range(2):
        Sc.op("dve", lambda e, i=i: e.tensor_tensor(out=junk, in0=lamb[:, (2 * i) * 128:(2 * i + 1) * 128],
                                                    in1=lamb[:, (2 * i + 1) * 128:(2 * i + 2) * 128], op=ALU.mult),
              reads=[lam_res], writes=[junk_res])
        Sc.op("dve", lambda e, i=i: e.reduce_sum(out=lsm[:, i:i + 1], in_=junk, axis=AX.X), reads=[junk_res],
              writes=[lam_res])
    Sc.op("act", lambda e: e.activation(out=lsm[:, 2:4], in_=lsm[:, 0:2], func=AF.Exp), reads=[lam_res],
          writes=[lam_res])
    Sc.op("dve", lambda e: e.tensor_tensor(out=lsm[:, 4:5], in0=lsm[:, 3:4], in1=lsm[:, 2:3], op=ALU.subtract),
          reads=[lam_res], writes=[lam_res])
    Sc.op("dve", lambda e: e.tensor_scalar(out=lsm[:, 5:6], in0=lsm[:, 4:5], scalar1=float(-lam_init), scalar2=None,
                                           op0=ALU.add), reads=[lam_res], writes=[lam_res])
    neglam = lsm[:, 5:6]
    offs = [-128, 0, 128, 256, 384]
    n = 0
    for h in range(4):
        for oi, o in enumerate(offs):
            build_bias_tile(Sc, cx, T.lineT5, T.r_lineT5, h, 1023 - o, biasB[0][:, h * 5 + oi, :], biasB[1],
                            mask_blocks(o, "B"), tl[n % 2])
            n += 1

    def load_head(h):
        qv = T.qkT_even[2048 + h * 256:2048 + (h + 1) * 256, :].rearrange("(m p) t -> p m t", p=128)
        kv = T.qkT_even[3072 + h * 256:3072 + (h + 1) * 256, :].rearrange("(m p) t -> p m t", p=128)
        Sc.dma("sp", QT[h % 2][0], qv, reads=[T.r_qkT], writes=[QT[h % 2][1]])
        Sc.dma("sp", KT[h % 2][0], kv, reads=[T.r_qkT], writes=[KT[h % 2][1]])
        Sc.dma("sp", V[h % 2][0], v3(T.v_b[h], 32), reads=[T.r_v], writes=[V[h % 2][1]])

    load_head(0)
    it = 0
    for h in range(4):
        if h + 1 < 4:
            load_head(h + 1)
        q_ap, q_res = QT[h % 2]
        k_ap, k_res = KT[h % 2]
        v_ap, v_res = V[h % 2]
        far_col = t5bc[:, 15 * 20 + h:15 * 20 + h + 1]
        for qt in range(NT):
            for m in range(2):
                kts = list(range(0, 4 * qt + 4))
                o0, o0r = cx.banks[2]
                o1b, o1r = cx.banks[3]
                db, dbr = cx.banks[4]
                for idx, kt in enumerate(kts):
                    oi = kt - (4 * qt - 1)
                    s_terms = [(k_ap[:, m, kt * 128:(kt + 1) * 128], q_ap[:, m, qt * 512:(qt + 1) * 512])]
                    s_reads = [k_res, q_res]
                    if oi >= 0:
                        s_terms.append((cx.ident_b, biasB[0][:, h * 5 + oi, :]))
                        s_reads += [biasB[1], cx.const_res]
                        bcol, bres = None, None
                    else:
                        bcol, bres = far_col, t5bc_res
                    pv = [(v_ap[:, kt, 0:128], v_res, o0, o0r), (v_ap[:, kt, 128:256], v_res, o1b, o1r),
                          (cx.ones_b, cx.const_res, db, dbr)]
                    attn_keytile(Sc, cx, s_terms, s_reads, bcol, bres, pv, idx == 0, idx == len(kts) - 1)
                Sc.op("dve", lambda e, db=db: e.reciprocal(out=rden[0], in_=db), reads=[dbr], writes=[rden[1]])
                if m == 0:
                    Sc.op("dve", lambda e, o0=o0: e.tensor_tensor(out=o1[0][:, 0, :], in0=o0, in1=rden[0], op=ALU.mult),
                          reads=[o0r, rden[1]], writes=[o1[1]])
                    Sc.op("dve", lambda e, o1b=o1b: e.tensor_tensor(out=o1[0][:, 1, :], in0=o1b, in1=rden[0],
                                                                    op=ALU.mult),
                          reads=[o1r, rden[1]], writes=[o1[1]])
                else:
                    for cc, (bk, bkr) in enumerate(((o0, o0r), (o1b, o1r))):
                        Sc.op("dve", lambda e, cc=cc, bk=bk: e.tensor_tensor(out=oo[0][:, cc, :], in0=bk, in1=rden[0],
                                                                             op=ALU.mult),
                              reads=[bkr, rden[1]], writes=[oo[1]])
                        Sc.op("dve", lambda e, cc=cc: e.scalar_tensor_tensor(
                            out=oo[0][:, cc, :], in0=oo[0][:, cc, :], scalar=neglam, in1=o1[0][:, cc, :],
                            op0=ALU.mult, op1=ALU.add), reads=[oo[1], o1[1], lam_res], writes=[oo[1]])
                        Sc.op("act", lambda e, cc=cc: e.activation(out=sqb[0][:, cc, :], in_=oo[0][:, cc, :],
                                                                   func=AF.Square), reads=[oo[1]], writes=[sqb[1]])
                    nb_, nbr = cx.banks[5]
                    mm_group(Sc, nb_, [(cx.ones_b, sqb[0][:, 0, :]), (cx.ones_b, sqb[0][:, 1, :])],
                             [sqb[1], cx.const_res], nbr)
                    Sc.op("act", lambda e, nb_=nb_: e.activation(out=rstd[0], in_=nb_, func=AF.Sqrt, bias=EPS,
                                                                 scale=1.0 / 256), reads=[nbr], writes=[rstd[1]])
                    Sc.op("dve", lambda e: e.reciprocal(out=rstd[0], in_=rstd[0]), reads=[rstd[1]], writes=[rstd[1]])
                    os_, osr = ost[it % 2]
                    it += 1
                    for cc in range(2):
                        Sc.op("dve", lambda e, cc=cc, os_=os_: e.scalar_tensor_tensor(
                            out=os_[:, cc, :], in0=oo[0][:, cc, :], scalar=gs[:, cc:cc + 1], in1=rstd[0],
                            op0=ALU.mult, op1=ALU.mult), reads=[oo[1], gs_res, rstd[1]], writes=[osr])
                    Sc.dma("pool", tile_view(T.mixT, qt)[:, 8 + h * 2:8 + h * 2 + 2, :], os_, reads=[osr],
                           writes=[T.r_mixT])
    Sc.barrier()


BIGNEG = -1.0e30
KNOCK = -2.0e30
TOPK = 256


def phase_indexer(Sc, cx, layer, T):
    A = cx.arena
    A.reset()
    cx.alloc_consts()
    kiT2 = (A.bf16(S), Res("kiT2"))
    Sc.dma("sp", kiT2[0], T.kiT, reads=[T.r_kiT], writes=[kiT2[1]])
    wi = (A.f32(256), Res("wi"))
    Sc.dma("sp", wi[0], T.wi_tm, reads=[T.r_wi], writes=[wi[1]])
    absw = (A.f32(256), Res("absw"))
    sgn = (A.f32(256), Res("sgn"))
    Sc.op("act", lambda e: e.activation(out=absw[0], in_=wi[0], func=AF.Abs), reads=[wi[1]], writes=[absw[1]])
    Sc.op("act", lambda e: e.activation(out=sgn[0], in_=wi[0], func=AF.Sign), reads=[wi[1]], writes=[sgn[1]])
    qi = [(v3(A.bf16(4 * 512), 4), Res("qi")) for _ in range(2)]
    sc = [(A.f32(S), Res("sc")) for _ in range(2)]
    rr = [(A.f32(512), Res("r")) for _ in range(3)]
    mx = (A.f32(8), Res("mx"))
    madd = [(A.bf16(S), Res("madd")) for _ in range(2)]
    mT = [(v3(A.bf16(4 * 128), 4), Res("mT")) for _ in range(3)]
    rri = 0
    mti = 0
    tb = [cx.banks[6], cx.banks[7]]
    tbi = 0

    def load_qi(qt):
        Sc.dma("pool", qi[qt % 2][0], T.qiT[:, qt * 512:(qt + 1) * 512].rearrange("(c p) t -> p c t", p=128),
               reads=[T.r_qiT], writes=[qi[qt % 2][1]])

    load_qi(0)
    for i in range(32):
        qt, qs = i // 4, i % 4
        if qs == 0 and qt + 1 < NT:
            load_qi(qt + 1)
        qi_ap, qi_res = qi[qt % 2]
        sc_ap, sc_res = sc[i % 2]
        nst = qt + 1
        ncol = 512 * nst
        nvalid = 128 * (i + 1)
        for st in range(nst):
            for h in range(8):
                pb, pr = cx.banks[(st * 8 + h) % 6]
                ph = (h % 2) * 64
                Sc.op("pe", lambda e, pb=pb, ph=ph, h=h, st=st, qs=qs, qi_ap=qi_ap: e.matmul(
                    pb, qi_ap[ph:ph + 64, h // 2, qs * 128:(qs + 1) * 128],
                    kiT2[0][ph:ph + 64, st * 512:(st + 1) * 512], start=True, stop=True),
                      reads=[qi_res, kiT2[1]], writes=[pr])
                r_ap, r_res = rr[rri % 3]
                rri += 1
                col = i * 8 + h
                Sc.op("act", lambda e, r_ap=r_ap, pb=pb, col=col: e.activation(
                    out=r_ap, in_=pb, func=AF.Relu, scale=absw[0][:, col:col + 1]), reads=[pr, absw[1]],
                      writes=[r_res])
                dst = sc_ap[:, st * 512:(st + 1) * 512]
                if h == 0:
                    Sc.op("pool", lambda e, dst=dst, r_ap=r_ap, col=col: e.tensor_scalar(
                        out=dst, in0=r_ap, scalar1=sgn[0][:, col:col + 1], scalar2=None, op0=ALU.mult),
                          reads=[r_res, sgn[1]], writes=[sc_res])
                else:
                    Sc.op("pool", lambda e, r_ap=r_ap, col=col: e.tensor_scalar(
                        out=r_ap, in0=r_ap, scalar1=sgn[0][:, col:col + 1], scalar2=None, op0=ALU.mult),
                          reads=[r_res, sgn[1]], writes=[r_res])
                    Sc.op("pool", lambda e, dst=dst, r_ap=r_ap: e.tensor_tensor(out=dst, in0=dst, in1=r_ap, op=ALU.add),
                          reads=[r_res], writes=[sc_res])
        if nvalid < ncol:
            Sc.op("pool", lambda e, sc_ap=sc_ap, nvalid=nvalid, ncol=ncol: e.memset(sc_ap[:, nvalid:ncol], BIGNEG),
                  reads=[], writes=[sc_res])
        Sc.op("pool", lambda e, sc_ap=sc_ap, nvalid=nvalid: e.memset(sc_ap[0:64, nvalid - 64:nvalid], BIGNEG),
              reads=[], writes=[sc_res])
        md_ap, md_res = madd[i % 2]
        if nvalid - 64 > TOPK:
            for rnd in range(TOPK // 8):
                Sc.op("dve", lambda e, sc_ap=sc_ap, nvalid=nvalid: e.max(out=mx[0], in_=sc_ap[:, 0:nvalid]),
                      reads=[sc_res], writes=[mx[1]])
                Sc.op("dve", lambda e, sc_ap=sc_ap, nvalid=nvalid: e.match_replace(
                    out=sc_ap[:, 0:nvalid], in_to_replace=mx[0], in_values=sc_ap[:, 0:nvalid], imm_value=KNOCK),
                      reads=[mx[1]], writes=[sc_res])
            Sc.op("dve", lambda e, md_ap=md_ap, sc_ap=sc_ap, ncol=ncol: e.tensor_scalar(
                out=md_ap[:, 0:ncol], in0=sc_ap[:, 0:ncol], scalar1=-1.5e30, scalar2=NEG, op0=ALU.is_ge, op1=ALU.mult),
                  reads=[sc_res], writes=[md_res])
        else:
            Sc.op("dve", lambda e, md_ap=md_ap, sc_ap=sc_ap, ncol=ncol: e.tensor_scalar(
                out=md_ap[:, 0:ncol], in0=sc_ap[:, 0:ncol], scalar1=-1.0e29, scalar2=NEG, op0=ALU.is_lt, op1=ALU.mult),
                  reads=[sc_res], writes=[md_res])
        for ktg in range(nst):
            bk, bkr = tb[tbi % 2]
            tbi += 1
            bkb = bk.bitcast(BF16)

            def tfn(e, bkb=bkb, md_ap=md_ap, ktg=ktg):
                ins = None
                for j in range(4):
                    kt = ktg * 4 + j
                    ins = e.transpose(bkb[:, j * 128:(j + 1) * 128], md_ap[:, kt * 128:(kt + 1) * 128], cx.ident_b)
                return ins

            Sc.op("pe", tfn, reads=[md_res, cx.const_res], writes=[bkr])
            mt_ap, mt_res = mT[mti % 3]
            mti += 1
            Sc.op("act", lambda e, mt_ap=mt_ap, bkb=bkb: e.activation(
                out=mt_ap, in_=bkb[:, 0:512].rearrange("p (c t) -> p c t", c=4), func=AF.Copy), reads=[bkr],
                  writes=[mt_res])
            dst = v3(T.maskT[qt], 32)[:, ktg * 4:(ktg + 1) * 4, qs * 128:(qs + 1) * 128]
            Sc.dma("pool", dst, mt_ap, reads=[mt_res], writes=[T.r_maskT])
    Sc.barrier()


def phase_attnC(Sc, cx, layer, T):
    o_ = layer // 2
    A = cx.arena
    A.reset()
    cx.alloc_consts()
    biasC = (v3(A.bf16(40 * 512), 40), Res("biasC"))
    tl = [(A.f32(512), Res("tl")) for _ in range(2)]
    clatT = (v3(A.bf16(2 * S), 2), Res("clatT"))
    clat = (v3(A.bf16(32 * 256), 32), Res("clat"))
    Sc.dma("pool", clatT[0], T.clatT.rearrange("(c p) t -> p c t", p=128), reads=[T.r_clatT], writes=[clatT[1]])
    Sc.dma("sp", clat[0], v3(T.clat_tm, 32), reads=[T.r_clat_tm], writes=[clat[1]])
    mk = (v3(A.bf16(32 * 512), 32), Res("mk"))
    qh = (v3(A.bf16(8 * 512), 8), Res("qh"))
    wuk = (v3(A.bf16(8 * 256), 8), Res("wuk"))
    wuv = (A.bf16(8 * 256).rearrange("p (h c d) -> p h c d", h=8, c=2), Res("wuv"))
    cx.ebufs = [(A.bf16(512), Res("E")) for _ in range(3)]
    cx.err = 0
    cx.sbanks = cx.banks[0:2]
    cx.srr = 0
    qlat = [(v3(A.bf16(2 * 512), 2), Res("qlat")) for _ in range(2)]
    olat = [(v3(A.bf16(2 * 512), 2), Res("olat")) for _ in range(2)]
    rden = (A.f32(512), Res("rden"))
    ost = [(A.bf16(512), Res("ost")) for _ in range(2)]
    t5bc = A.f32(640)
    t5bc_res = Res("t5bc")
    Sc.dma("sp", t5bc, T.t5_bc, reads=[T.r_small], writes=[t5bc_res])
    offs = [-128, 0, 128, 256, 384]
    it = 0
    for hg in range(2):
        Sc.dma("pool", wuk[0], T.w_ukT[o_, hg * 8:(hg + 1) * 8].rearrange("h p c -> p h c"), reads=[T.r_w],
               writes=[wuk[1]])
        Sc.dma("pool", wuv[0], T.w_uv[o_, hg * 8:(hg + 1) * 8].rearrange("h p (c d) -> p h c d", c=2),
               reads=[T.r_w], writes=[wuv[1]])
        n = 0
        for hl in range(8):
            for oi, o in enumerate(offs):
                build_bias_tile(Sc, cx, T.lineT5, T.r_lineT5, 4 + hg * 8 + hl, 1023 - o, biasC[0][:, hl * 5 + oi, :],
                                biasC[1], [], tl[n % 2])
                n += 1
        for qt in range(NT):
            nkt = 4 * qt + 4
            Sc.dma("sp", mk[0][:, 0:nkt, :], v3(T.maskT[qt], 32)[:, 0:nkt, :], reads=[T.r_maskT], writes=[mk[1]])
            Sc.dma("pool", qh[0], T.qcT[hg * 1024:(hg + 1) * 1024, qt * 512:(qt + 1) * 512].rearrange(
                "(h p) t -> p h t", p=128), reads=[T.r_qcT], writes=[qh[1]])
            for hl in range(8):
                h = hg * 8 + hl
                ql, qlr = qlat[it % 2]
                ol, olr = olat[it % 2]
                os_, osr = ost[it % 2]
                it += 1
                for cc in range(2):
                    pb, pr = cx.banks[5 + cc]
                    mm_group(Sc, pb, [(wuk[0][:, hl, cc * 128:(cc + 1) * 128], qh[0][:, hl, :])], [wuk[1], qh[1]], pr)
                    evac(Sc, cx, ql[:, cc, :], pb, [pr], [qlr], SCALE)
                far_col = t5bc[:, 15 * 20 + 4 + h:15 * 20 + 4 + h + 1]
                o0, o0r = cx.banks[2]
                o1b, o1r = cx.banks[3]
                db, dbr = cx.banks[4]
                for kt in range(nkt):
                    oi = kt - (4 * qt - 1)
                    s_terms = [(clatT[0][:, 0, kt * 128:(kt + 1) * 128], ql[:, 0, :]),
                               (clatT[0][:, 1, kt * 128:(kt + 1) * 128], ql[:, 1, :]),
                               (cx.ident_b, mk[0][:, kt, :])]
                    s_reads = [clatT[1], qlr, mk[1], cx.const_res]
                    if oi >= 0:
                        s_terms.append((cx.ident_b, biasC[0][:, hl * 5 + oi, :]))
                        s_reads.append(biasC[1])
                        bcol, bres = None, None
                    else:
                        bcol, bres = far_col, t5bc_res
                    pv = [(clat[0][:, kt, 0:128], clat[1], o0, o0r), (clat[0][:, kt, 128:256], clat[1], o1b, o1r),
                          (cx.ones_b, cx.const_res, db, dbr)]
                    attn_keytile(Sc, cx, s_terms, s_reads, bcol, bres, pv, kt == 0, kt == nkt - 1)
                Sc.op("dve", lambda e, db=db: e.reciprocal(out=rden[0], in_=db), reads=[dbr], writes=[rden[1]])
                for cc, (bk, bkr) in enumerate(((o0, o0r), (o1b, o1r))):
                    Sc.op("dve", lambda e, cc=cc, bk=bk, ol=ol: e.tensor_tensor(out=ol[:, cc, :], in0=bk, in1=rden[0],
                                                                                op=ALU.mult),
                          reads=[bkr, rden[1]], writes=[olr])
                ub, ubr = cx.banks[7]
                mm_group(Sc, ub, [(wuv[0][:, hl, cc, :], ol[:, cc, :]) for cc in range(2)], [wuv[1], olr], ubr)
                evac(Sc, cx, os_, ub, [ubr], [osr])
                Sc.dma("pool", tile_view(T.mixT, qt)[:, h, :], os_, reads=[osr], writes=[T.r_mixT])
    Sc.barrier()


def phase_ffn(Sc, cx, layer, T):
    even = (layer % 2 == 0)
    last = (layer == DEPTH - 1)
    A = cx.arena
    A.reset()
    cx.alloc_consts()
    xt = (v3(A.f32(16 * 512), 16), Res("xt"))
    mixb = (v3(A.bf16(16 * 512), 16), Res("mix"))
    hTb = (v3(A.bf16(16 * 512), 16), Res("hT"))
    aT = (v3(A.bf16(NJ * 512), NJ), Res("aT"))
    cx.sq = [(A.f32(512), Res("sq")) for _ in range(4)]
    cx.rstd = (A.f32(512), Res("rstd"))
    wo_buf = [(A.bf16(16 * 256), Res("wb")) for _ in range(4)]
    wd_buf = [(A.bf16(NJ * 128), Res("wd")) for _ in range(2)]
    sg = [(A.f32(512), Res("sg")) for _ in range(2)]
    g = A.f32(16)
    g_res = Res("g")
    Sc.dma("sp", g, T.ffn_norm[layer], reads=[T.r_small], writes=[g_res])
    if last:
        gf = A.f32(16)
        gf_res = Res("gf")
        Sc.dma("sp", gf, T.final_norm, reads=[T.r_small], writes=[gf_res])
    first = (layer == 0 or T.force_x_in)
    xsrc = T.x_in if first else T.xs
    xsrc_res = T.r_x_in if first else T.r_xs
    Wo = T.w_out_even[layer // 2] if even else T.w_out_odd[layer // 2]
    Wg, Wu, Wd = T.w_gate[layer], T.w_up[layer], T.w_down[layer]
    blocks = []
    for t in range(NT):
        blocks += [Wo[nb] for nb in range(8)]
        for jb in range(NJ // 2):
            blocks += [Wg[jb], Wu[jb]]
    PER = 8 + NJ
    ws = WStream(Sc, wo_buf, blocks, T.r_w, 2, hold=2)
    wds = WStream(Sc, wd_buf, [Wd[nb] for t in range(NT) for nb in range(16)], T.r_w, 1)
    ws.ensure(2)
    for t in range(NT):
        x_ap, x_res = xt
        Sc.dma("sp", x_ap, tile_view(xsrc, t), reads=[xsrc_res], writes=[x_res])
        Sc.dma("sp", mixb[0], tile_view(T.mixT, t), reads=[T.r_mixT], writes=[mixb[1]])
        for nb in range(8):
            wb, wres = ws.get(t * PER + nb)
            wv = v3(wb, 16)
            for sub in range(2):
                n = nb * 2 + sub
                pb, pr = cx.bank()
                mm_group(Sc, pb, [(wv[:, c, sub * 128:(sub + 1) * 128], mixb[0][:, c, :]) for c in range(16)],
                         [wres, mixb[1]], pr)
                Sc.op("dve", lambda e, n=n, pb=pb: e.tensor_tensor(out=x_ap[:, n, :], in0=x_ap[:, n, :], in1=pb,
                                                                   op=ALU.add), reads=[pr, x_res], writes=[x_res])
        hT, hT_res = hTb
        rmsnorm_fm(Sc, cx, x_ap, x_res, hT, hT_res, g, g_res, 16, D)
        for jb in range(NJ // 2):
            wgb, wgres = ws.get(t * PER + 8 + 2 * jb)
            wub, wures = ws.get(t * PER + 8 + 2 * jb + 1)
            wgv, wuv = v3(wgb, 16), v3(wub, 16)
            if jb == NJ // 2 - 4:
                wds.ensure(t * 16 + 1)
            for sub in range(2):
                j = jb * 2 + sub
                pg, pgr = cx.bank()
                mm_group(Sc, pg, [(wgv[:, c, sub * 128:(sub + 1) * 128], hT[:, c, :]) for c in range(16)],
                         [wgres, hT_res], pgr)
                pu, pur = cx.bank()
                mm_group(Sc, pu, [(wuv[:, c, sub * 128:(sub + 1) * 128], hT[:, c, :]) for c in range(16)],
                         [wures, hT_res], pur)
                sgb, sgr = sg[j % 2]
                Sc.op("act", lambda e, sgb=sgb, pg=pg: e.activation(out=sgb, in_=pg, func=AF.Silu), reads=[pgr],
                      writes=[sgr])
                Sc.op("dve", lambda e, j=j, sgb=sgb, pu=pu: e.tensor_tensor(out=aT[0][:, j, :], in0=sgb, in1=pu,
                                                                            op=ALU.mult),
                      reads=[sgr, pur], writes=[aT[1]])
        for nb in range(16):
            buf, res = wds.get(t * 16 + nb)
            wdv = v3(buf, NJ)
            pb, pr = cx.bank()
            mm_group(Sc, pb, [(wdv[:, j, :], aT[0][:, j, :]) for j in range(NJ)], [res, aT[1]], pr)
            Sc.op("dve", lambda e, nb=nb, pb=pb: e.tensor_tensor(out=x_ap[:, nb, :], in0=x_ap[:, nb, :], in1=pb,
                                                                 op=ALU.add), reads=[pr, x_res], writes=[x_res])
        if not last:
            Sc.dma("sp", tile_view(T.xs, t), x_ap, reads=[x_res], writes=[T.r_xs])
        else:
            rmsnorm_fm(Sc, cx, x_ap, x_res, x_ap, x_res, gf, gf_res, 16, D)
            Sc.dma("sp", tile_view(T.out, t), x_ap, reads=[x_res], writes=[T.r_out])
    Sc.barrier()


INPUT_SPECS = [
    ("x_in", [NT, 128, 16 * 512]),
    ("attn_norm", [DEPTH, 128, 16]),
    ("ffn_norm", [DEPTH, 128, 16]),
    ("final_norm", [128, 16]),
    ("w_in_even", [2, 12, 128, 16 * 512]),
    ("w_in_odd", [2, 6, 128, 16 * 512]),
    ("w_out_even", [2, 8, 128, 16 * 256]),
    ("w_out_odd", [2, 8, 128, 16 * 256]),
    ("w_gate", [DEPTH, NJ // 2, 128, 16 * 256]),
    ("w_up", [DEPTH, NJ // 2, 128, 16 * 256]),
    ("w_down", [DEPTH, 16, 128, NJ * 128]),
    ("c_kv_norm_fm", [2, 128, 2]),
    ("c_kv_norm_bc", [2, 128, 256]),
    ("ident", [128, 128]),
    ("a_rel_T", [2, 8, 257]),
    ("t5_T", [20, 32]),
    ("t5_bc", [128, 640]),
    ("b_lambda_bc", [2, 128, 512]),
    ("b_subln_fm", [2, 128, 2]),
    ("w_ukT", [2, 16, 128, 256]),
    ("w_uv", [2, 16, 128, 2 * 128]),
]


def build(cfg):
    nc = bass.Bass("TRN2", target_bir_lowering=False)
    T = Ctx()
    T.force_x_in = bool(cfg.get("force_x_in", False))
    feed = set(cfg.get("feed", ()))
    dump = set(cfg.get("dump", ()))

    def dram(name, shape, dtype, kind):
        return nc.dram_tensor(name, list(shape), dtype, kind=kind).ap()

    def scratch(name, shape, dtype):
        kind = "Internal"
        if name in feed:
            kind = "ExternalInput"
        elif name in dump:
            kind = "ExternalOutput"
        return dram(name, shape, dtype, kind)

    for (name, shape) in INPUT_SPECS:
        setattr(T, name, dram(name, shape, F32, "ExternalInput"))
    T.out = dram("out", [NT, 128, 16 * 512], F32, "ExternalOutput")
    T.r_x_in, T.r_small, T.r_w, T.r_out = Res(), Res(), Res(), Res()
    T.xs = scratch("xs", [NT, 128, 16 * 512], F32)
    T.qkT_even = scratch("qkT_even", [4096, S], BF16)
    T.v_a = scratch("v_a", [8, 128, 32 * 128], BF16)
    T.v_b = scratch("v_b", [4, 128, 32 * 256], BF16)
    T.mixT = scratch("mixT", [NT, 128, 16 * 512], BF16)
    T.qcT = scratch("qcT", [2048, S], BF16)
    T.clatT = scratch("clatT", [256, S], BF16)
    T.clat_tm = scratch("clat_tm", [128, 32 * 256], BF16)
    T.qiT = scratch("qiT", [512, S], BF16)
    T.kiT = scratch("kiT", [128, S], BF16)
    T.wi_tm = scratch("wi_tm", [128, 32 * 8], F32)
    T.lineA = scratch("lineA", [8, 128 * 2048], F32)
    T.lineT5 = scratch("lineT5", [20, 128 * 2048], F32)
    T.maskT = scratch("maskT", [NT, 128, 32 * 512], BF16)
    for nm in ("xs", "qkT", "v", "mixT", "qcT", "clatT", "clat_tm", "qiT", "kiT", "wi", "lineA", "lineT5", "maskT"):
        setattr(T, "r_" + nm, Res(nm))

    with ExitStack() as stack:
        big = stack.enter_context(nc.sbuf_tensor("big", [128, ARENA_WORDS], F32))
        banks = [stack.enter_context(nc.psum_tensor("bank%d" % i, [128, 512], F32)) for i in range(8)]
        Sc = Sched(nc, stack)
        cx = Ctx()
        cx.arena = Arena(big)
        cx.banks = [(b[:, :], Res("bank%d" % i)) for i, b in enumerate(banks)]
        cx.brr = 0
        cx.evrr = 0
        cx.const_res = Res("const")

        def bank():
            i = cx.brr % 8
            cx.brr += 1
            return cx.banks[i]

        cx.bank = bank

        def alloc_consts():
            A = cx.arena
            cx.ones_f = A.f32(128)
            cx.ones_b = A.bf16(128)
            cx.ident_b = A.bf16(128)
            Sc.op("pool", lambda e: e.memset(cx.ones_f, 1.0), writes=[cx.const_res])
            Sc.op("pool", lambda e: e.memset(cx.ones_b, 1.0), writes=[cx.const_res])
            Sc.dma("pool", cx.ident_b, T.ident, reads=[T.r_small], writes=[cx.const_res])

        cx.alloc_consts = alloc_consts

        for (ph, layer) in cfg["phases"]:
            if ph == "inproj":
                phase_inproj(Sc, cx, layer, T)
            elif ph == "ffn":
                phase_ffn(Sc, cx, layer, T)
            elif ph == "setup_t5":
                phase_setup_t5(Sc, cx, T)
            elif ph == "attnA":
                phase_attnA(Sc, cx, layer, T)
            elif ph == "attnB":
                phase_attnB(Sc, cx, layer, T)
            elif ph == "idx":
                phase_indexer(Sc, cx, layer, T)
            elif ph == "attnC":
                phase_attnC(Sc, cx, layer, T)
            else:
                raise ValueError(ph)
        Sc.finish("sp")

        with nc.Block() as block:
            @block.tensor
            def _(e):
                Sc.replay("pe", e)

            @block.scalar
            def _(e):
                Sc.replay("act", e)

            @block.vector
            def _(e):
                Sc.replay("dve", e)

            @block.gpsimd
            def _(e):
                Sc.replay("pool", e)

            @block.sync
            def _(e):
                Sc.replay("sp", e)
    return nc, Sc


def _blk_cols(W, width, npad=None):
    K, N = W.shape
    if npad is not None and npad > N:
        W = np.concatenate([W, np.zeros((K, npad - N), W.dtype)], axis=1)
        N = npad
    kc = K // 128
    a = W.reshape(kc, 128, N // width, width).transpose(2, 1, 0, 3)
    return np.ascontiguousarray(a).reshape(N // width, 128, kc * width)


def _vec_fm(v):
    n = v.shape[-1] // 128
    return np.ascontiguousarray(np.swapaxes(v.reshape(v.shape[:-1] + (n, 128)), -1, -2))


def _tile_act(xb):
    a = xb.reshape(NT, 512, 16, 128).transpose(0, 3, 2, 1)
    return np.ascontiguousarray(a).reshape(NT, 128, 16 * 512)


def _untile_act(o):
    a = o.reshape(NT, 128, 16, 512).transpose(0, 3, 2, 1)
    return np.ascontiguousarray(a).reshape(S, D)


def prep_shared(inp):
    f = lambda a: np.asarray(a, dtype=np.float32)
    sh = {}
    sh["attn_norm"] = _vec_fm(f(inp["attn_norm"]))
    sh["ffn_norm"] = _vec_fm(f(inp["ffn_norm"]))
    sh["final_norm"] = _vec_fm(f(inp["final_norm"]))
    sh["w_in_even"] = np.stack([_blk_cols(f(inp["even_w_in"][i]), 512) for i in range(2)])
    sh["w_in_odd"] = np.stack([_blk_cols(f(inp["odd_w_in"][i]), 512, 3072) for i in range(2)])
    sh["w_out_even"] = np.stack([_blk_cols(f(inp["even_w_out"][i]), 256) for i in range(2)])
    sh["w_out_odd"] = np.stack([_blk_cols(f(inp["odd_w_out"][i]), 256) for i in range(2)])
    sh["w_gate"] = np.stack([_blk_cols(f(inp["w_gate"][i]), 256) for i in range(DEPTH)])
    sh["w_up"] = np.stack([_blk_cols(f(inp["w_up"][i]), 256) for i in range(DEPTH)])
    sh["w_down"] = np.stack([_blk_cols(f(inp["w_down"][i]), 128) for i in range(DEPTH)])
    ck = f(inp["c_kv_norm"])
    sh["c_kv_norm_fm"] = _vec_fm(ck)
    sh["c_kv_norm_bc"] = np.ascontiguousarray(np.broadcast_to(ck[:, None, :], (2, 128, 256)))
    sh["ident"] = np.eye(128, dtype=np.float32)
    sh["a_rel_T"] = np.ascontiguousarray(f(inp["a_rel_bias"]).transpose(0, 2, 1))
    t5 = f(inp["t5_table"])
    sh["t5_T"] = np.ascontiguousarray(t5.T)
    sh["t5_bc"] = np.ascontiguousarray(np.broadcast_to(t5.reshape(1, 640), (128, 640)))
    bl = f(inp["b_lambda"]).reshape(2, 1, 512)
    sh["b_lambda_bc"] = np.ascontiguousarray(np.broadcast_to(bl, (2, 128, 512)))
    sh["b_subln_fm"] = _vec_fm(f(inp["b_subln"]))
    sh["w_ukT"] = np.ascontiguousarray(f(inp["c_w_uk"]).transpose(0, 1, 3, 2))
    wuv = f(inp["c_w_uv"]).reshape(2, 16, 2, 128, 128).transpose(0, 1, 3, 2, 4)
    sh["w_uv"] = np.ascontiguousarray(wuv).reshape(2, 16, 128, 256)
    return sh


def full_phases():
    ph = [("setup_t5", 0)]
    for layer in range(DEPTH):
        ph.append(("inproj", layer))
        if layer % 2 == 0:
            ph += [("attnA", layer), ("attnB", layer)]
        else:
            ph += [("idx", layer), ("attnC", layer)]
        ph.append(("ffn", layer))
    return ph


def kernel(**inputs):
    sh = prep_shared(inputs)
    x = np.asarray(inputs["x"], dtype=np.float32)
    nc, _ = build({"phases": full_phases()})
    in_maps = []
    for b in range(8):
        m = dict(sh)
        m["x_in"] = _tile_act(x[b])
        in_maps.append(m)
    res = run_bass_kernel_spmd(nc, in_maps, core_ids=list(range(8)))
    out = np.stack([_untile_act(np.asarray(r["out"], dtype=np.float32)) for r in res.results])
    return out
```

```python
import math
from contextlib import ExitStack

import numpy as np
import concourse.bass as bass
import concourse.mybir as mybir
from concourse.bass_utils import run_bass_kernel_spmd

F32 = mybir.dt.float32
BF16 = mybir.dt.bfloat16
AF = mybir.ActivationFunctionType
ALU = mybir.AluOpType
AX = mybir.AxisListType

D = 2048
S = 4096
DEPTH = 4
NT = S // 512
DFF = 5632
NJ = DFF // 128
EPS = 1e-6
NEG = -30000.0
SCALE = 128 ** -0.5
ODD_PROJ = 2888
ARENA_WORDS = 51200
FFN_NBUF = 4


class Res:
    __slots__ = ("name", "w", "r")

    def __init__(self, name=""):
        self.name = name
        self.w = None
        self.r = {}


class Sched:
    ENGS = ("pe", "act", "dve", "pool", "sp")
    NDMA = 8

    def __init__(self, nc, stack):
        self.nc = nc
        self.q = {e: [] for e in self.ENGS}
        self.sem = {e: stack.enter_context(nc.semaphore("s_" + e)) for e in self.ENGS}
        self.cnt = {e: 0 for e in self.ENGS}
        self.known = {e: {} for e in self.ENGS}
        self.pending = {e: {} for e in self.ENGS}
        self.dsem = {}
        self.dcnt = {}
        self.drr = {}
        for qn in ("sp", "pool", "act"):
            nd = self.NDMA
            self.dsem[qn] = [stack.enter_context(nc.semaphore("d_%s%d" % (qn, i))) for i in range(nd)]
            self.drr[qn] = 0
            for s in self.dsem[qn]:
                self.dcnt[id(s)] = 0
        self.nops = 0

    def _collect(self, eng, reads, writes, acc):
        waits = dict(self.pending[eng])
        self.pending[eng] = {}
        kn = self.known[eng]
        own = self.sem[eng] if eng in self.sem else None

        def need(ev):
            if ev is None:
                return
            s, v = ev
            k = id(s)
            if kn.get(k, 0) >= v:
                return
            if k not in waits or waits[k][1] < v:
                waits[k] = (s, v)

        for r in reads:
            need(r.w)
        for w in writes:
            if not (acc and w.w is not None and w.w[0] is own):
                need(w.w)
            for ev in w.r.values():
                need(ev)
        return waits

    def _commit(self, eng, reads, writes, ev, waits):
        kn = self.known[eng]
        for k, (s, v) in waits.items():
            if kn.get(k, 0) < v:
                kn[k] = v
        k = id(ev[0])
        for r in reads:
            r.r[k] = ev
        for w in writes:
            w.w = ev
            w.r = {}

    def op(self, eng, fn, reads=(), writes=(), acc=False):
        waits = self._collect(eng, reads, writes, acc)
        self.cnt[eng] += 1
        ev = (self.sem[eng], self.cnt[eng])
        self._commit(eng, reads, writes, ev, waits)
        self.q[eng].append((list(waits.values()), fn, (self.sem[eng], 1)))
        self.nops += 1
        return ev

    def dma(self, qn, out_ap, in_ap, reads=(), writes=()):
        eng = qn
        i = self.drr[qn] % len(self.dsem[qn])
        self.drr[qn] += 1
        s = self.dsem[qn][i]
        waits = self._collect(eng, reads, writes, False)
        prev = self.dcnt[id(s)]
        if prev > 0 and self.known[eng].get(id(s), 0) < prev:
            waits[id(s)] = (s, prev)
        self.dcnt[id(s)] = prev + 16
        ev = (s, prev + 16)
        self._commit(eng, reads, writes, ev, waits)

        def fn(e, out_ap=out_ap, in_ap=in_ap):
            return e.dma_start(out=out_ap, in_=in_ap)

        self.q[eng].append((list(waits.values()), fn, (s, 16)))
        self.nops += 1
        return ev

    def barrier(self):
        evs = []
        for e in self.ENGS:
            if self.cnt[e] > 0:
                evs.append((self.sem[e], self.cnt[e]))
        for qn in self.dsem:
            for s in self.dsem[qn]:
                if self.dcnt[id(s)] > 0:
                    evs.append((s, self.dcnt[id(s)]))
        for e in self.ENGS:
            kn = self.known[e]
            for (s, v) in evs:
                if kn.get(id(s), 0) < v:
                    p = self.pending[e]
                    if id(s) not in p or p[id(s)][1] < v:
                        p[id(s)] = (s, v)

    def finish(self, eng="sp"):
        self.barrier()
        waits = self.pending[eng]
        self.pending[eng] = {}
        self.q[eng].append((list(waits.values()), None, None))

    def replay(self, eng, e):
        for (waits, fn, inc) in self.q[eng]:
            for (s, v) in waits:
                e.wait_ge(s, v)
            if fn is not None:
                ins = fn(e)
                ins.then_inc(inc[0], inc[1])


class Arena:
    def __init__(self, big):
        self.big = big
        self.off = 0

    def reset(self):
        self.off = 0

    def f32(self, n):
        assert self.off + n <= ARENA_WORDS, "arena overflow %d" % (self.off + n)
        ap = self.big[:, self.off:self.off + n]
        self.off += n
        return ap

    def bf16(self, n):
        w = (n + 1) // 2
        ap = self.f32(w).bitcast(BF16)
        return ap[:, 0:n]


class Ctx:
    pass


def v3(ap, c):
    return ap.rearrange("p (c t) -> p c t", c=c)


def mm_group(Sc, out_ap, terms, reads, out_res):
    terms = list(terms)

    def fn(e):
        n = len(terms)
        ins = None
        for i, (l, r) in enumerate(terms):
            ins = e.matmul(out_ap, l, r, start=(i == 0), stop=(i == n - 1))
        return ins

    return Sc.op("pe", fn, reads=reads, writes=[out_res])


def rmsnorm_fm(Sc, cx, xt, xt_res, hT, hT_res, g, g_res, nchunk, dim, out_f32=None):
    pb, pr = cx.bank()
    for c in range(nchunk):
        sq, sqr = cx.sq[c % len(cx.sq)]
        Sc.op("act", lambda e, o=sq, i=xt[:, c, :]: e.activation(out=o, in_=i, func=AF.Square),
              reads=[xt_res], writes=[sqr])
        Sc.op("pe", lambda e, o=pb, r=sq, c=c: e.matmul(o, cx.ones_b[:, :], r, start=(c == 0), stop=(c == nchunk - 1)),
              reads=[sqr, cx.const_res], writes=[pr], acc=(c > 0))
    rstd, rr = cx.rstd
    Sc.op("act", lambda e: e.activation(out=rstd, in_=pb, func=AF.Sqrt, bias=EPS, scale=1.0 / dim),
          reads=[pr], writes=[rr])
    Sc.op("dve", lambda e: e.reciprocal(out=rstd, in_=rstd), reads=[rr], writes=[rr])
    for c in range(nchunk):
        Sc.op("dve", lambda e, c=c: e.scalar_tensor_tensor(out=hT[:, c, :], in0=xt[:, c, :], scalar=g[:, c:c + 1],
                                                            in1=rstd, op0=ALU.mult, op1=ALU.mult),
              reads=[xt_res, g_res, rr], writes=[hT_res])


class WStream:
    def __init__(self, Sc, bufs, blocks, r_w, pf, hold=1):
        self.Sc, self.bufs, self.blocks, self.r_w, self.pf = Sc, bufs, blocks, r_w, pf
        self.next = 0
        assert pf <= len(bufs) - hold

    def ensure(self, k):
        while self.next < len(self.blocks) and self.next <= k:
            buf, res = self.bufs[self.next % len(self.bufs)]
            blk = self.blocks[self.next]
            n = blk.shape[1]
            self.Sc.dma("pool", buf[:, 0:n], blk, reads=[self.r_w], writes=[res])
            self.next += 1

    def get(self, i):
        self.ensure(i + self.pf)
        return self.bufs[i % len(self.bufs)]


def evac(Sc, cx, out_ap, in_ap, reads, writes, scale=None):
    cx.evrr += 1
    if cx.evrr % 2 == 0:
        if scale is None:
            Sc.op("act", lambda e: e.activation(out=out_ap, in_=in_ap, func=AF.Copy), reads=reads, writes=writes)
        else:
            Sc.op("act", lambda e: e.activation(out=out_ap, in_=in_ap, func=AF.Copy, scale=float(scale)),
                  reads=reads, writes=writes)
    else:
        if scale is None:
            Sc.op("dve", lambda e: e.tensor_copy(out=out_ap, in_=in_ap), reads=reads, writes=writes)
        else:
            Sc.op("dve", lambda e: e.tensor_scalar(out=out_ap, in0=in_ap, scalar1=float(scale), scalar2=None,
                                                   op0=ALU.mult), reads=reads, writes=writes)


T5_EDGES = [1, 2, 3, 4, 5, 6, 7, 8, 12, 16, 23, 32, 46, 64, 91]


def t5_bucket_static(rel):
    n = abs(rel)
    b = sum(1 for e in T5_EDGES if e <= n)
    return b + (16 if rel > 0 else 0)


def tile_view(t_ap, t):
    return v3(t_ap[t], 16)


def phase_inproj(Sc, cx, layer, T):
    even = (layer % 2 == 0)
    A = cx.arena
    A.reset()
    cx.alloc_consts()
    xt = [(v3(A.f32(16 * 512), 16), Res("xt")) for _ in range(2)]
    hTs = [(v3(A.bf16(16 * 512), 16), Res("hT")) for _ in range(2)]
    cx.sq = [(A.bf16(512), Res("sq")) for _ in range(4)]
    cx.rstd = (A.f32(512), Res("rstd"))
    wbufs = [(A.bf16(16 * 512), Res("wb")) for _ in range(3)]
    stage = [(v3(A.bf16(4 * 512), 4), Res("stg")) for _ in range(3)]
    srr = [0]

    def get_stage():
        s_ = stage[srr[0] % 3]
        srr[0] += 1
        return s_

    g = A.f32(16)
    g_res = Res("g")
    Sc.dma("sp", g, T.attn_norm[layer], reads=[T.r_small], writes=[g_res])
    if not even:
        o = layer // 2
        gk = A.f32(2)
        gk_res = Res("gk")
        Sc.dma("sp", gk, T.c_kv_norm_fm[o], reads=[T.r_small], writes=[gk_res])
        gkb = A.f32(256)
        gkb_res = Res("gkb")
        Sc.dma("sp", gkb, T.c_kv_norm_bc[o], reads=[T.r_small], writes=[gkb_res])
        clf = (v3(A.f32(2 * 512), 2), Res("clf"))
        clo = (v3(A.bf16(2 * 512), 2), Res("clo"))
        cltm = (A.f32(256), Res("cltm"))
        cltm_o = (v3(A.bf16(4 * 256), 4), Res("cltmo"))
        ssq = (A.f32(4), Res("ssq"))
        junk = (A.f32(256), Res("junk"))
        wstage = (v3(A.f32(4 * 8), 4), Res("wst"))
    first = (layer == 0 or T.force_x_in)
    xsrc = T.x_in if first else T.xs
    xsrc_res = T.r_x_in if first else T.r_xs
    W = T.w_in_even[layer // 2] if even else T.w_in_odd[layer // 2]
    nblk = 12 if even else 6
    ws = WStream(Sc, wbufs, [W[nb] for t in range(NT) for nb in range(nblk)], T.r_w, 2)

    def load_x(t):
        Sc.dma("sp", xt[t % 2][0], tile_view(xsrc, t), reads=[xsrc_res], writes=[xt[t % 2][1]])

    load_x(0)
    ws.ensure(1)
    for t in range(NT):
        if t + 1 < NT:
            load_x(t + 1)
        x_ap, x_res = xt[t % 2]
        hT, hT_res = hTs[t % 2]
        rmsnorm_fm(Sc, cx, x_ap, x_res, hT, hT_res, g, g_res, 16, D)
        tok = slice(t * 512, (t + 1) * 512)
        for nb in range(nblk):
            wb, wres = ws.get(t * nblk + nb)
            wv = v3(wb, 16)
            if even:
                kind = "tm" if nb in (4, 5, 10, 11) else "fm"
            else:
                kind = "fm" if nb < 4 else ("odd4" if nb == 4 else "odd5")
            if kind == "fm":
                st, st_res = get_stage()
                scale = SCALE if (even and nb in (0, 1, 6, 7)) else None
                for sub in range(4):
                    pb, pr = cx.bank()
                    mm_group(Sc, pb, [(wv[:, c, sub * 128:(sub + 1) * 128], hT[:, c, :]) for c in range(16)],
                             [wres, hT_res], pr)
                    evac(Sc, cx, st[:, sub, :], pb, [pr], [st_res], scale)
                if even:
                    r0 = _even_fm_row(nb)
                    dst = T.qkT_even[r0:r0 + 512, tok]
                    dres = T.r_qkT
                else:
                    dst = T.qcT[nb * 512:(nb + 1) * 512, tok]
                    dres = T.r_qcT
                Sc.dma("pool", dst.rearrange("(s p) t -> p s t", p=128), st, reads=[st_res], writes=[dres])
            elif kind == "tm":
                st, st_res = get_stage()
                for sub in range(4):
                    pb, pr = cx.bank()
                    mm_group(Sc, pb, [(hT[:, c, sub * 128:(sub + 1) * 128], wv[:, c, :]) for c in range(16)],
                             [wres, hT_res], pr)
                    evac(Sc, cx, st[:, sub, :], pb, [pr], [st_res])
                if nb in (4, 5):
                    for hh in range(4):
                        h = (nb - 4) * 4 + hh
                        Sc.dma("pool", v3(T.v_a[h], 32)[:, t * 4:(t + 1) * 4, :], st[:, :, hh * 128:(hh + 1) * 128],
                               reads=[st_res], writes=[T.r_v])
                else:
                    for hh in range(2):
                        h = (nb - 10) * 2 + hh
                        Sc.dma("pool", v3(T.v_b[h], 32)[:, t * 4:(t + 1) * 4, :], st[:, :, hh * 256:(hh + 1) * 256],
                               reads=[st_res], writes=[T.r_v])
            elif kind == "odd4":
                clf_ap, clf_res = clf
                for cc in range(2):
                    pb, pr = cx.bank()
                    mm_group(Sc, pb, [(wv[:, c, cc * 128:(cc + 1) * 128], hT[:, c, :]) for c in range(16)],
                             [wres, hT_res], pr)
                    evac(Sc, cx, clf_ap[:, cc, :], pb, [pr], [clf_res])
                rmsnorm_fm(Sc, cx, clf_ap, clf_res, clo[0], clo[1], gk, gk_res, 2, 256)
                Sc.dma("pool", T.clatT[:, tok].rearrange("(s p) t -> p s t", p=128), clo[0], reads=[clo[1]],
                       writes=[T.r_clatT])
                for sub in range(4):
                    pb, pr = cx.bank()
                    mm_group(Sc, pb[:, 0:256],
                             [(hT[:, c, sub * 128:(sub + 1) * 128], wv[:, c, 0:256]) for c in range(16)],
                             [wres, hT_res], pr)
                    Sc.op("dve", lambda e, pb=pb: e.tensor_copy(out=cltm[0], in_=pb[:, 0:256]), reads=[pr],
                          writes=[cltm[1]])
                    Sc.op("act", lambda e: e.activation(out=junk[0], in_=cltm[0], func=AF.Square,
                                                        accum_out=ssq[0][:, 0:1]),
                          reads=[cltm[1]], writes=[junk[1], ssq[1]])
                    Sc.op("act", lambda e: e.activation(out=ssq[0][:, 1:2], in_=ssq[0][:, 0:1], func=AF.Sqrt, bias=EPS,
                                                        scale=1.0 / 256), reads=[ssq[1]], writes=[ssq[1]])
                    Sc.op("dve", lambda e: e.reciprocal(out=ssq[0][:, 2:3], in_=ssq[0][:, 1:2]), reads=[ssq[1]],
                          writes=[ssq[1]])
                    Sc.op("dve", lambda e, sub=sub: e.scalar_tensor_tensor(
                        out=cltm_o[0][:, sub, :], in0=cltm[0], scalar=ssq[0][:, 2:3], in1=gkb,
                        op0=ALU.mult, op1=ALU.mult), reads=[cltm[1], ssq[1], gkb_res], writes=[cltm_o[1]])
                Sc.dma("pool", v3(T.clat_tm, 32)[:, t * 4:(t + 1) * 4, :], cltm_o[0], reads=[cltm_o[1]],
                       writes=[T.r_clat_tm])
                st, st_res = get_stage()
                for sub in range(2):
                    pb, pr = cx.bank()
                    mm_group(Sc, pb,
                             [(wv[:, c, 256 + sub * 128:256 + (sub + 1) * 128], hT[:, c, :]) for c in range(16)],
                             [wres, hT_res], pr)
                    evac(Sc, cx, st[:, sub, :], pb, [pr], [st_res])
                Sc.dma("pool", T.qiT[0:256, tok].rearrange("(s p) t -> p s t", p=128), st[:, 0:2, :], reads=[st_res],
                       writes=[T.r_qiT])
            else:
                st, st_res = get_stage()
                for sub in range(2):
                    pb, pr = cx.bank()
                    mm_group(Sc, pb, [(wv[:, c, sub * 128:(sub + 1) * 128], hT[:, c, :]) for c in range(16)],
                             [wres, hT_res], pr)
                    evac(Sc, cx, st[:, sub, :], pb, [pr], [st_res])
                pb, pr = cx.bank()
                mm_group(Sc, pb[0:64, :], [(wv[:, c, 256:320], hT[:, c, :]) for c in range(16)], [wres, hT_res], pr)
                evac(Sc, cx, st[0:64, 2, :], pb[0:64, :], [pr], [st_res])
                Sc.dma("pool", T.qiT[256:512, tok].rearrange("(s p) t -> p s t", p=128), st[:, 0:2, :], reads=[st_res],
                       writes=[T.r_qiT])
                Sc.dma("pool", T.kiT[0:64, tok], st[0:64, 2, :], reads=[st_res], writes=[T.r_kiT])
                Sc.dma("pool", T.kiT[64:128, tok], st[0:64, 2, :], reads=[st_res], writes=[T.r_kiT])
                for sub in range(4):
                    pb, pr = cx.bank()
                    mm_group(Sc, pb[:, 0:8],
                             [(hT[:, c, sub * 128:(sub + 1) * 128], wv[:, c, 320:328]) for c in range(16)],
                             [wres, hT_res], pr)
                    Sc.op("dve", lambda e, pb=pb, sub=sub: e.tensor_copy(out=wstage[0][:, sub, :], in_=pb[:, 0:8]),
                          reads=[pr], writes=[wstage[1]])
                Sc.dma("pool", v3(T.wi_tm, 32)[:, t * 4:(t + 1) * 4, :], wstage[0], reads=[wstage[1]], writes=[T.r_wi])
    Sc.barrier()


def _even_fm_row(nb):
    return {0: 0, 1: 512, 2: 1024, 3: 1536, 6: 2048, 7: 2560, 8: 3072, 9: 3584}[nb]


def build_line_buffer(Sc, cx, tabT, tab_res, nh, segs, B_dram, B_res, fline):
    f_ap, f_res = fline
    for (a, b, src, rng) in segs:
        if rng:
            Sc.op("dve", lambda e, a=a, b=b, src=src: e.tensor_copy(out=f_ap[0:nh, a:b], in_=tabT[0:nh, src:src + (b - a)]),
                  reads=[tab_res], writes=[f_res])
        else:
            Sc.op("dve", lambda e, a=a, b=b, src=src: e.tensor_copy(
                out=f_ap[0:nh, a:b], in_=tabT[0:nh, src:src + 1].to_broadcast([nh, b - a])),
                  reads=[tab_res], writes=[f_res])
    pstep = f_ap.ap[0][0]
    src_ap = bass.AP(f_ap.tensor, f_ap.offset, [[pstep, nh], [0, 128], [1, 2048]])
    Sc.dma("pool", B_dram.rearrange("h (r m) -> h r m", m=2048), src_ap, reads=[f_res], writes=[B_res])


def build_bias_tile(Sc, cx, B_dram, B_res, h, base, dst_ap, dst_res, masked_blocks, tl):
    tl_ap, tl_res = tl
    src = bass.AP(B_dram.tensor, h * 128 * 2048 + base, [[2047, 128], [1, 512]])
    Sc.dma("pool", tl_ap, src, reads=[B_res], writes=[tl_res])
    for (a, b0, b1) in masked_blocks:
        Sc.op("pool", lambda e, a=a, b0=b0, b1=b1: e.memset(tl_ap[a * 64:(a + 1) * 64, b0 * 64:b1 * 64], NEG),
              reads=[], writes=[tl_res])
    Sc.op("act", lambda e: e.activation(out=dst_ap, in_=tl_ap, func=AF.Copy), reads=[tl_res], writes=[dst_res])


def mask_blocks(o, rule):
    out = []
    for a in range(2):
        masked = []
        for b in range(8):
            delta = o // 64 + a - b
            if rule == "A":
                ok = (-8 <= delta <= 0)
            elif rule == "B":
                ok = (delta <= 0)
            else:
                ok = True
            masked.append(not ok)
        b = 0
        while b < 8:
            if masked[b]:
                b1 = b
                while b1 < 8 and masked[b1]:
                    b1 += 1
                out.append((a, b, b1))
                b = b1
            else:
                b += 1
    return out


def attn_keytile(Sc, cx, s_terms, s_reads, bias_col, bias_res, pv, first, last):
    sb, sr = cx.sbanks[cx.srr % 2]
    cx.srr += 1
    mm_group(Sc, sb, s_terms, s_reads, sr)
    E, Er = cx.ebufs[cx.err % len(cx.ebufs)]
    cx.err += 1
    if bias_col is None:
        Sc.op("act", lambda e: e.activation(out=E, in_=sb, func=AF.Exp), reads=[sr], writes=[Er])
    else:
        Sc.op("act", lambda e: e.activation(out=E, in_=sb, func=AF.Exp, bias=bias_col), reads=[sr, bias_res],
              writes=[Er])
    for (l, lres, acc_ap, acc_res) in pv:
        Sc.op("pe", lambda e, l=l, acc_ap=acc_ap: e.matmul(acc_ap, l, E, start=first, stop=last),
              reads=[Er, lres, cx.const_res], writes=[acc_res], acc=(not first))


def phase_attnA(Sc, cx, layer, T):
    e_ = layer // 2
    A = cx.arena
    A.reset()
    cx.alloc_consts()
    biasA = (v3(A.bf16(64 * 512), 64), Res("biasA"))
    tl = [(A.f32(512), Res("tl")) for _ in range(2)]
    tabT = A.f32(260)
    tab_res = Res("tab")
    fline = (A.f32(2048), Res("fline"))
    QT = [(A.bf16(S), Res("QT")) for _ in range(2)]
    KT = [(A.bf16(S), Res("KT")) for _ in range(2)]
    V = [(v3(A.bf16(32 * 128), 32), Res("V")) for _ in range(2)]
    cx.ebufs = [(A.bf16(512), Res("E")) for _ in range(3)]
    cx.err = 0
    rden = [(A.f32(512), Res("rden")) for _ in range(2)]
    ost = [(A.bf16(512), Res("ost")) for _ in range(2)]
    cx.sbanks = cx.banks[0:2]
    cx.srr = 0
    Sc.dma("sp", tabT[0:8, 0:257], T.a_rel_T[e_], reads=[T.r_small], writes=[tab_res])
    segs = [(0, 384, 0, False), (384, 641, 0, True), (641, 2048, 256, False)]
    build_line_buffer(Sc, cx, tabT, tab_res, 8, segs, T.lineA, T.r_lineA, fline)
    offs = [-512, -384, -256, -128, 0, 128, 256, 384]
    n = 0
    for h in range(8):
        for oi, o in enumerate(offs):
            build_bias_tile(Sc, cx, T.lineA, T.r_lineA, h, 512 - o, biasA[0][:, h * 8 + oi, :], biasA[1],
                            mask_blocks(o, "A"), tl[n % 2])
            n += 1

    def load_head(h):
        Sc.dma("sp", QT[h % 2][0], T.qkT_even[h * 128:(h + 1) * 128, :], reads=[T.r_qkT], writes=[QT[h % 2][1]])
        Sc.dma("sp", KT[h % 2][0], T.qkT_even[1024 + h * 128:1024 + (h + 1) * 128, :], reads=[T.r_qkT],
               writes=[KT[h % 2][1]])
        Sc.dma("sp", V[h % 2][0], v3(T.v_a[h], 32), reads=[T.r_v], writes=[V[h % 2][1]])

    load_head(0)
    it = 0
    for h in range(8):
        if h + 1 < 8:
            load_head(h + 1)
        q_ap, q_res = QT[h % 2]
        k_ap, k_res = KT[h % 2]
        v_ap, v_res = V[h % 2]
        for qt in range(NT):
            kts = [kt for kt in range(4 * qt - 4, 4 * qt + 4) if kt >= 0]
            ob, obr = cx.banks[2 + (it % 2) * 2]
            db, dbr = cx.banks[3 + (it % 2) * 2]
            it += 1
            for idx, kt in enumerate(kts):
                oi = kt - (4 * qt - 4)
                s_terms = [(k_ap[:, kt * 128:(kt + 1) * 128], q_ap[:, qt * 512:(qt + 1) * 512]),
                           (cx.ident_b, biasA[0][:, h * 8 + oi, :])]
                pv = [(v_ap[:, kt, :], v_res, ob, obr), (cx.ones_b, cx.const_res, db, dbr)]
                attn_keytile(Sc, cx, s_terms, [k_res, q_res, biasA[1], cx.const_res], None, None, pv,
                             idx == 0, idx == len(kts) - 1)
            rd, rdr = rden[it % 2]
            os_, osr = ost[it % 2]
            Sc.op("dve", lambda e, rd=rd, db=db: e.reciprocal(out=rd, in_=db), reads=[dbr], writes=[rdr])
            Sc.op("dve", lambda e, os_=os_, ob=ob, rd=rd: e.tensor_tensor(out=os_, in0=ob, in1=rd, op=ALU.mult),
                  reads=[obr, rdr], writes=[osr])
            Sc.dma("pool", tile_view(T.mixT, qt)[:, h, :], os_, reads=[osr], writes=[T.r_mixT])
    Sc.barrier()


def t5_line_segs():
    segs = []
    m = 0
    while m < 2048:
        b = t5_bucket_static(1023 - m)
        m1 = m
        while m1 < 2048 and t5_bucket_static(1023 - m1) == b:
            m1 += 1
        segs.append((m, m1, b, False))
        m = m1
    return segs


def phase_setup_t5(Sc, cx, T):
    A = cx.arena
    A.reset()
    cx.alloc_consts()
    tabT = A.f32(32)
    tab_res = Res("tab")
    fline = (A.f32(2048), Res("fline"))
    Sc.dma("sp", tabT[0:20, 0:32], T.t5_T, reads=[T.r_small], writes=[tab_res])
    build_line_buffer(Sc, cx, tabT, tab_res, 20, t5_line_segs(), T.lineT5, T.r_lineT5, fline)
    Sc.barrier()


def phase_attnB(Sc, cx, layer, T):
    e_ = layer // 2
    lam_init = 0.8 - 0.6 * math.exp(-0.3 * layer)
    A = cx.arena
    A.reset()
    cx.alloc_consts()
    biasB = (v3(A.bf16(20 * 512), 20), Res("biasB"))
    tl = [(A.f32(512), Res("tl")) for _ in range(2)]
    QT = [(v3(A.bf16(2 * S), 2), Res("QT")) for _ in range(2)]
    KT = [(v3(A.bf16(2 * S), 2), Res("KT")) for _ in range(2)]
    V = [(v3(A.bf16(32 * 256), 32), Res("V")) for _ in range(2)]
    cx.ebufs = [(A.bf16(512), Res("E")) for _ in range(3)]
    cx.err = 0
    cx.sbanks = cx.banks[0:2]
    cx.srr = 0
    rden = (A.f32(512), Res("rden"))
    o1 = (v3(A.f32(2 * 512), 2), Res("o1"))
    oo = (v3(A.f32(2 * 512), 2), Res("oo"))
    sqb = (v3(A.bf16(2 * 512), 2), Res("sqb"))
    rstd = (A.f32(512), Res("rstdB"))
    ost = [(v3(A.bf16(2 * 512), 2), Res("ost")) for _ in range(2)]
    t5bc = A.f32(640)
    t5bc_res = Res("t5bc")
    Sc.dma("sp", t5bc, T.t5_bc, reads=[T.r_small], writes=[t5bc_res])
    lamb = A.f32(512)
    lam_res = Res("lam")
    Sc.dma("sp", lamb, T.b_lambda_bc[e_], reads=[T.r_small], writes=[lam_res])
    lsm = A.f32(8)
    junk = A.f32(128)
    junk_res = Res("junk")
    gs = A.f32(2)
    gs_res = Res("gs")
    Sc.dma("sp", gs, T.b_subln_fm[e_], reads=[T.r_small], writes=[gs_res])
    Sc.op("dve", lambda e: e.tensor_scalar(out=gs, in0=gs, scalar1=float(1.0 - lam_init), scalar2=None, op0=ALU.mult),
          reads=[gs_res], writes=[gs_res])
    for i in range(2):
        Sc.op("dve", lambda e, i=i: e.tensor_tensor(out=junk, in0=lamb[:, (2 * i) * 128:(2 * i + 1) * 128],
                                                    in1=lamb[:, (2 * i + 1) * 128:(2 * i + 2) * 128], op=ALU.mult),
              reads=[lam_res], writes=[junk_res])
        Sc.op("dve", lambda e, i=i: e.reduce_sum(out=lsm[:, i:i + 1], in_=junk, axis=AX.X), reads=[junk_res],
              writes=[lam_res])
    Sc.op("act", lambda e: e.activation(out=lsm[:, 2:4], in_=lsm[:, 0:2], func=AF.Exp), reads=[lam_res],
          writes=[lam_res])
    Sc.op("dve", lambda e: e.tensor_tensor(out=lsm[:, 4:5], in0=lsm[:, 3:4], in1=lsm[:, 2:3], op=ALU.subtract),
          reads=[lam_res], writes=[lam_res])
    Sc.op("dve", lambda e: e.tensor_scalar(out=lsm[:, 5:6], in0=lsm[:, 4:5], scalar1=float(-lam_init), scalar2=None,
                                           op0=ALU.add), reads=[lam_res], writes=[lam_res])
    neglam = lsm[:, 5:6]
    offs = [-128, 0, 128, 256, 384]
    n = 0
    for h in range(4):
        for oi, o in enumerate(offs):
            build_bias_tile(Sc, cx, T.lineT5, T.r_lineT5, h, 1023 - o, biasB[0][:, h * 5 + oi, :], biasB[1],
                            mask_blocks(o, "B"), tl[n % 2])
            n += 1

    def load_head(h):
        qv = T.qkT_even[2048 + h * 256:2048 + (h + 1) * 256, :].rearrange("(m p) t -> p m t", p=128)
        kv = T.qkT_even[3072 + h * 256:3072 + (h + 1) * 256, :].rearrange("(m p) t -> p m t", p=128)
        Sc.dma("sp", QT[h % 2][0], qv, reads=[T.r_qkT], writes=[QT[h % 2][1]])
        Sc.dma("sp", KT[h % 2][0], kv, reads=[T.r_qkT], writes=[KT[h % 2][1]])
        Sc.dma("sp", V[h % 2][0], v3(T.v_b[h], 32), reads=[T.r_v], writes=[V[h % 2][1]])

    load_head(0)
    it = 0
    for h in range(4):
        if h + 1 < 4:
            load_head(h + 1)
        q_ap, q_res = QT[h % 2]
        k_ap, k_res = KT[h % 2]
        v_ap, v_res = V[h % 2]
        far_col = t5bc[:, 15 * 20 + h:15 * 20 + h + 1]
        for qt in range(NT):
            for m in range(2):
                kts = list(range(0, 4 * qt + 4))
                o0, o0r = cx.banks[2]
                o1b, o1r = cx.banks[3]
                db, dbr = cx.banks[4]
                for idx, kt in enumerate(kts):
                    oi = kt - (4 * qt - 1)
                    s_terms = [(k_ap[:, m, kt * 128:(kt + 1) * 128], q_ap[:, m, qt * 512:(qt + 1) * 512])]
                    s_reads = [k_res, q_res]
                    if oi >= 0:
                        s_terms.append((cx.ident_b, biasB[0][:, h * 5 + oi, :]))
                        s_reads += [biasB[1], cx.const_res]
                        bcol, bres = None, None
                    else:
                        bcol, bres = far_col, t5bc_res
                    pv = [(v_ap[:, kt, 0:128], v_res, o0, o0r), (v_ap[:, kt, 128:256], v_res, o1b, o1r),
                          (cx.ones_b, cx.const_res, db, dbr)]
                    attn_keytile(Sc, cx, s_terms, s_reads, bcol, bres, pv, idx == 0, idx == len(kts) - 1)
                Sc.op("dve", lambda e, db=db: e.reciprocal(out=rden[0], in_=db), reads=[dbr], writes=[rden[1]])
                if m == 0:
                    Sc.op("dve", lambda e, o0=o0: e.tensor_tensor(out=o1[0][:, 0, :], in0=o0, in1=rden[0], op=ALU.mult),
                          reads=[o0r, rden[1]], writes=[o1[1]])
                    Sc.op("dve", lambda e, o1b=o1b: e.tensor_tensor(out=o1[0][:, 1, :], in0=o1b, in1=rden[0],
                                                                    op=ALU.mult),
                          reads=[o1r, rden[1]], writes=[o1[1]])
                else:
                    for cc, (bk, bkr) in enumerate(((o0, o0r), (o1b, o1r))):
                        Sc.op("dve", lambda e, cc=cc, bk=bk: e.tensor_tensor(out=oo[0][:, cc, :], in0=bk, in1=rden[0],
                                                                             op=ALU.mult),
                              reads=[bkr, rden[1]], writes=[oo[1]])
                        Sc.op("dve", lambda e, cc=cc: e.scalar_tensor_tensor(
                            out=oo[0][:, cc, :], in0=oo[0][:, cc, :], scalar=neglam, in1=o1[0][:, cc, :],
                            op0=ALU.mult, op1=ALU.add), reads=[oo[1], o1[1], lam_res], writes=[oo[1]])
                        Sc.op("act", lambda e, cc=cc: e.activation(out=sqb[0][:, cc, :], in_=oo[0][:, cc, :],
                                                                   func=AF.Square), reads=[oo[1]], writes=[sqb[1]])
                    nb_, nbr = cx.banks[5]
                    mm_group(Sc, nb_, [(cx.ones_b, sqb[0][:, 0, :]), (cx.ones_b, sqb[0][:, 1, :])],
                             [sqb[1], cx.const_res], nbr)
                    Sc.op("act", lambda e, nb_=nb_: e.activation(out=rstd[0], in_=nb_, func=AF.Sqrt, bias=EPS,
                                                                 scale=1.0 / 256), reads=[nbr], writes=[rstd[1]])
                    Sc.op("dve", lambda e: e.reciprocal(out=rstd[0], in_=rstd[0]), reads=[rstd[1]], writes=[rstd[1]])
                    os_, osr = ost[it % 2]
                    it += 1
                    for cc in range(2):
                        Sc.op("dve", lambda e, cc=cc, os_=os_: e.scalar_tensor_tensor(
                            out=os_[:, cc, :], in0=oo[0][:, cc, :], scalar=gs[:, cc:cc + 1], in1=rstd[0],
                            op0=ALU.mult, op1=ALU.mult), reads=[oo[1], gs_res, rstd[1]], writes=[osr])
                    Sc.dma("pool", tile_view(T.mixT, qt)[:, 8 + h * 2:8 + h * 2 + 2, :], os_, reads=[osr],
                           writes=[T.r_mixT])
    Sc.barrier()


BIGNEG = -1.0e30
KNOCK = -2.0e30
TOPK = 256


def phase_indexer(Sc, cx, layer, T):
    A = cx.arena
    A.reset()
    cx.alloc_consts()
    kiT2 = (A.bf16(S), Res("kiT2"))
    Sc.dma("sp", kiT2[0], T.kiT, reads=[T.r_kiT], writes=[kiT2[1]])
    wi = (A.f32(256), Res("wi"))
    Sc.dma("sp", wi[0], T.wi_tm, reads=[T.r_wi], writes=[wi[1]])
    absw = (A.f32(256), Res("absw"))
    sgn = (A.f32(256), Res("sgn"))
    Sc.op("act", lambda e: e.activation(out=absw[0], in_=wi[0], func=AF.Abs), reads=[wi[1]], writes=[absw[1]])
    Sc.op("act", lambda e: e.activation(out=sgn[0], in_=wi[0], func=AF.Sign), reads=[wi[1]], writes=[sgn[1]])
    qi = [(v3(A.bf16(4 * 512), 4), Res("qi")) for _ in range(2)]
    sc = [(A.f32(S), Res("sc")) for _ in range(2)]
    rr = [(A.f32(512), Res("r")) for _ in range(3)]
    mx = (A.f32(8), Res("mx"))
    madd = [(A.bf16(S), Res("madd")) for _ in range(2)]
    mT = [(v3(A.bf16(4 * 128), 4), Res("mT")) for _ in range(3)]
    rri = 0
    mti = 0
    tb = [cx.banks[6], cx.banks[7]]
    tbi = 0

    def load_qi(qt):
        Sc.dma("pool", qi[qt % 2][0], T.qiT[:, qt * 512:(qt + 1) * 512].rearrange("(c p) t -> p c t", p=128),
               reads=[T.r_qiT], writes=[qi[qt % 2][1]])

    load_qi(0)
    for i in range(32):
        qt, qs = i // 4, i % 4
        if qs == 0 and qt + 1 < NT:
            load_qi(qt + 1)
        qi_ap, qi_res = qi[qt % 2]
        sc_ap, sc_res = sc[i % 2]
        nst = qt + 1
        ncol = 512 * nst
        nvalid = 128 * (i + 1)
        for st in range(nst):
            for h in range(8):
                pb, pr = cx.banks[(st * 8 + h) % 6]
                ph = (h % 2) * 64
                Sc.op("pe", lambda e, pb=pb, ph=ph, h=h, st=st, qs=qs, qi_ap=qi_ap: e.matmul(
                    pb, qi_ap[ph:ph + 64, h // 2, qs * 128:(qs + 1) * 128],
                    kiT2[0][ph:ph + 64, st * 512:(st + 1) * 512], start=True, stop=True),
                      reads=[qi_res, kiT2[1]], writes=[pr])
                r_ap, r_res = rr[rri % 3]
                rri += 1
                col = i * 8 + h
                Sc.op("act", lambda e, r_ap=r_ap, pb=pb, col=col: e.activation(
                    out=r_ap, in_=pb, func=AF.Relu, scale=absw[0][:, col:col + 1]), reads=[pr, absw[1]],
                      writes=[r_res])
                dst = sc_ap[:, st * 512:(st + 1) * 512]
                if h == 0:
                    Sc.op("pool", lambda e, dst=dst, r_ap=r_ap, col=col: e.tensor_scalar(
                        out=dst, in0=r_ap, scalar1=sgn[0][:, col:col + 1], scalar2=None, op0=ALU.mult),
                          reads=[r_res, sgn[1]], writes=[sc_res])
                else:
                    Sc.op("pool", lambda e, r_ap=r_ap, col=col: e.tensor_scalar(
                        out=r_ap, in0=r_ap, scalar1=sgn[0][:, col:col + 1], scalar2=None, op0=ALU.mult),
                          reads=[r_res, sgn[1]], writes=[r_res])
                    Sc.op("pool", lambda e, dst=dst, r_ap=r_ap: e.tensor_tensor(out=dst, in0=dst, in1=r_ap, op=ALU.add),
                          reads=[r_res], writes=[sc_res])
        if nvalid < ncol:
            Sc.op("pool", lambda e, sc_ap=sc_ap, nvalid=nvalid, ncol=ncol: e.memset(sc_ap[:, nvalid:ncol], BIGNEG),
                  reads=[], writes=[sc_res])
        Sc.op("pool", lambda e, sc_ap=sc_ap, nvalid=nvalid: e.memset(sc_ap[0:64, nvalid - 64:nvalid], BIGNEG),
              reads=[], writes=[sc_res])
        md_ap, md_res = madd[i % 2]
        if nvalid - 64 > TOPK:
            for rnd in range(TOPK // 8):
                Sc.op("dve", lambda e, sc_ap=sc_ap, nvalid=nvalid: e.max(out=mx[0], in_=sc_ap[:, 0:nvalid]),
                      reads=[sc_res], writes=[mx[1]])
                Sc.op("dve", lambda e, sc_ap=sc_ap, nvalid=nvalid: e.match_replace(
                    out=sc_ap[:, 0:nvalid], in_to_replace=mx[0], in_values=sc_ap[:, 0:nvalid], imm_value=KNOCK),
                      reads=[mx[1]], writes=[sc_res])
            Sc.op("dve", lambda e, md_ap=md_ap, sc_ap=sc_ap, ncol=ncol: e.tensor_scalar(
                out=md_ap[:, 0:ncol], in0=sc_ap[:, 0:ncol], scalar1=-1.5e30, scalar2=NEG, op0=ALU.is_ge, op1=ALU.mult),
                  reads=[sc_res], writes=[md_res])
        else:
            Sc.op("dve", lambda e, md_ap=md_ap, sc_ap=sc_ap, ncol=ncol: e.tensor_scalar(
                out=md_ap[:, 0:ncol], in0=sc_ap[:, 0:ncol], scalar1=-1.0e29, scalar2=NEG, op0=ALU.is_lt, op1=ALU.mult),
                  reads=[sc_res], writes=[md_res])
        for ktg in range(nst):
            bk, bkr = tb[tbi % 2]
            tbi += 1
            bkb = bk.bitcast(BF16)

            def tfn(e, bkb=bkb, md_ap=md_ap, ktg=ktg):
                ins = None
                for j in range(4):
                    kt = ktg * 4 + j
                    ins = e.transpose(bkb[:, j * 128:(j + 1) * 128], md_ap[:, kt * 128:(kt + 1) * 128], cx.ident_b)
                return ins

            Sc.op("pe", tfn, reads=[md_res, cx.const_res], writes=[bkr])
            mt_ap, mt_res = mT[mti % 3]
            mti += 1
            Sc.op("act", lambda e, mt_ap=mt_ap, bkb=bkb: e.activation(
                out=mt_ap, in_=bkb[:, 0:512].rearrange("p (c t) -> p c t", c=4), func=AF.Copy), reads=[bkr],
                  writes=[mt_res])
            dst = v3(T.maskT[qt], 32)[:, ktg * 4:(ktg + 1) * 4, qs * 128:(qs + 1) * 128]
            Sc.dma("pool", dst, mt_ap, reads=[mt_res], writes=[T.r_maskT])
    Sc.barrier()


def phase_attnC(Sc, cx, layer, T):
    o_ = layer // 2
    A = cx.arena
    A.reset()
    cx.alloc_consts()
    biasC = (v3(A.bf16(40 * 512), 40), Res("biasC"))
    tl = [(A.f32(512), Res("tl")) for _ in range(2)]
    clatT = (v3(A.bf16(2 * S), 2), Res("clatT"))
    clat = (v3(A.bf16(32 * 256), 32), Res("clat"))
    Sc.dma("pool", clatT[0], T.clatT.rearrange("(c p) t -> p c t", p=128), reads=[T.r_clatT], writes=[clatT[1]])
    Sc.dma("sp", clat[0], v3(T.clat_tm, 32), reads=[T.r_clat_tm], writes=[clat[1]])
    mk = (v3(A.bf16(32 * 512), 32), Res("mk"))
    qh = (v3(A.bf16(8 * 512), 8), Res("qh"))
    wuk = (v3(A.bf16(8 * 256), 8), Res("wuk"))
    wuv = (A.bf16(8 * 256).rearrange("p (h c d) -> p h c d", h=8, c=2), Res("wuv"))
    cx.ebufs = [(A.bf16(512), Res("E")) for _ in range(3)]
    cx.err = 0
    cx.sbanks = cx.banks[0:2]
    cx.srr = 0
    qlat = [(v3(A.bf16(2 * 512), 2), Res("qlat")) for _ in range(2)]
    olat = [(v3(A.bf16(2 * 512), 2), Res("olat")) for _ in range(2)]
    rden = (A.f32(512), Res("rden"))
    ost = [(A.bf16(512), Res("ost")) for _ in range(2)]
    t5bc = A.f32(640)
    t5bc_res = Res("t5bc")
    Sc.dma("sp", t5bc, T.t5_bc, reads=[T.r_small], writes=[t5bc_res])
    offs = [-128, 0, 128, 256, 384]
    it = 0
    for hg in range(2):
        Sc.dma("pool", wuk[0], T.w_ukT[o_, hg * 8:(hg + 1) * 8].rearrange("h p c -> p h c"), reads=[T.r_w],
               writes=[wuk[1]])
        Sc.dma("pool", wuv[0], T.w_uv[o_, hg * 8:(hg + 1) * 8].rearrange("h p (c d) -> p h c d", c=2),
               reads=[T.r_w], writes=[wuv[1]])
        n = 0
        for hl in range(8):
            for oi, o in enumerate(offs):
                build_bias_tile(Sc, cx, T.lineT5, T.r_lineT5, 4 + hg * 8 + hl, 1023 - o, biasC[0][:, hl * 5 + oi, :],
                                biasC[1], [], tl[n % 2])
                n += 1
        for qt in range(NT):
            nkt = 4 * qt + 4
            Sc.dma("sp", mk[0][:, 0:nkt, :], v3(T.maskT[qt], 32)[:, 0:nkt, :], reads=[T.r_maskT], writes=[mk[1]])
            Sc.dma("pool", qh[0], T.qcT[hg * 1024:(hg + 1) * 1024, qt * 512:(qt + 1) * 512].rearrange(
                "(h p) t -> p h t", p=128), reads=[T.r_qcT], writes=[qh[1]])
            for hl in range(8):
                h = hg * 8 + hl
                ql, qlr = qlat[it % 2]
                ol, olr = olat[it % 2]
                os_, osr = ost[it % 2]
                it += 1
                for cc in range(2):
                    pb, pr = cx.banks[5 + cc]
                    mm_group(Sc, pb, [(wuk[0][:, hl, cc * 128:(cc + 1) * 128], qh[0][:, hl, :])], [wuk[1], qh[1]], pr)
                    evac(Sc, cx, ql[:, cc, :], pb, [pr], [qlr], SCALE)
                far_col = t5bc[:, 15 * 20 + 4 + h:15 * 20 + 4 + h + 1]
                o0, o0r = cx.banks[2]
                o1b, o1r = cx.banks[3]
                db, dbr = cx.banks[4]
                for kt in range(nkt):
                    oi = kt - (4 * qt - 1)
                    s_terms = [(clatT[0][:, 0, kt * 128:(kt + 1) * 128], ql[:, 0, :]),
                               (clatT[0][:, 1, kt * 128:(kt + 1) * 128], ql[:, 1, :]),
                               (cx.ident_b, mk[0][:, kt, :])]
                    s_reads = [clatT[1], qlr, mk[1], cx.const_res]
                    if oi >= 0:
                        s_terms.append((cx.ident_b, biasC[0][:, hl * 5 + oi, :]))
                        s_reads.append(biasC[1])
                        bcol, bres = None, None
                    else:
                        bcol, bres = far_col, t5bc_res
                    pv = [(clat[0][:, kt, 0:128], clat[1], o0, o0r), (clat[0][:, kt, 128:256], clat[1], o1b, o1r),
                          (cx.ones_b, cx.const_res, db, dbr)]
                    attn_keytile(Sc, cx, s_terms, s_reads, bcol, bres, pv, kt == 0, kt == nkt - 1)
                Sc.op("dve", lambda e, db=db: e.reciprocal(out=rden[0], in_=db), reads=[dbr], writes=[rden[1]])
                for cc, (bk, bkr) in enumerate(((o0, o0r), (o1b, o1r))):
                    Sc.op("dve", lambda e, cc=cc, bk=bk, ol=ol: e.tensor_tensor(out=ol[:, cc, :], in0=bk, in1=rden[0],
                                                                                op=ALU.mult),
                          reads=[bkr, rden[1]], writes=[olr])
                ub, ubr = cx.banks[7]
                mm_group(Sc, ub, [(wuv[0][:, hl, cc, :], ol[:, cc, :]) for cc in range(2)], [wuv[1], olr], ubr)
                evac(Sc, cx, os_, ub, [ubr], [osr])
                Sc.dma("pool", tile_view(T.mixT, qt)[:, h, :], os_, reads=[osr], writes=[T.r_mixT])
    Sc.barrier()


def phase_ffn(Sc, cx, layer, T):
    even = (layer % 2 == 0)
    last = (layer == DEPTH - 1)
    A = cx.arena
    A.reset()
    cx.alloc_consts()
    xt = (v3(A.f32(16 * 512), 16), Res("xt"))
    mixb = (v3(A.bf16(16 * 512), 16), Res("mix"))
    hTb = (v3(A.bf16(16 * 512), 16), Res("hT"))
    aT = (v3(A.bf16(NJ * 512), NJ), Res("aT"))
    cx.sq = [(A.bf16(512), Res("sq")) for _ in range(4)]
    cx.rstd = (A.f32(512), Res("rstd"))
    wo_buf = [(A.bf16(16 * 256), Res("wb")) for _ in range(FFN_NBUF)]
    wd_buf = [(A.bf16(NJ * 128), Res("wd")) for _ in range(2)]
    sg = [(A.f32(512), Res("sg")) for _ in range(2)]
    g = A.f32(16)
    g_res = Res("g")
    Sc.dma("sp", g, T.ffn_norm[layer], reads=[T.r_small], writes=[g_res])
    if last:
        gf = A.f32(16)
        gf_res = Res("gf")
        Sc.dma("sp", gf, T.final_norm, reads=[T.r_small], writes=[gf_res])
    first = (layer == 0 or T.force_x_in)
    xsrc = T.x_in if first else T.xs
    xsrc_res = T.r_x_in if first else T.r_xs
    Wo = T.w_out_even[layer // 2] if even else T.w_out_odd[layer // 2]
    Wg, Wu, Wd = T.w_gate[layer], T.w_up[layer], T.w_down[layer]
    blocks = []
    for t in range(NT):
        blocks += [Wo[nb] for nb in range(8)]
        for jb in range(NJ // 2):
            blocks += [Wg[jb], Wu[jb]]
    PER = 8 + NJ
    ws = WStream(Sc, wo_buf, blocks, T.r_w, FFN_NBUF - 2, hold=2)
    wds = WStream(Sc, wd_buf, [Wd[nb] for t in range(NT) for nb in range(16)], T.r_w, 1)
    ws.ensure(2)
    for t in range(NT):
        x_ap, x_res = xt
        Sc.dma("sp", x_ap, tile_view(xsrc, t), reads=[xsrc_res], writes=[x_res])
        Sc.dma("sp", mixb[0], tile_view(T.mixT, t), reads=[T.r_mixT], writes=[mixb[1]])
        for nb in range(8):
            wb, wres = ws.get(t * PER + nb)
            wv = v3(wb, 16)
            for sub in range(2):
                n = nb * 2 + sub
                pb, pr = cx.bank()
                mm_group(Sc, pb, [(wv[:, c, sub * 128:(sub + 1) * 128], mixb[0][:, c, :]) for c in range(16)],
                         [wres, mixb[1]], pr)
                Sc.op("dve", lambda e, n=n, pb=pb: e.tensor_tensor(out=x_ap[:, n, :], in0=x_ap[:, n, :], in1=pb,
                                                                   op=ALU.add), reads=[pr, x_res], writes=[x_res])
        hT, hT_res = hTb
        rmsnorm_fm(Sc, cx, x_ap, x_res, hT, hT_res, g, g_res, 16, D)
        for jb in range(NJ // 2):
            wgb, wgres = ws.get(t * PER + 8 + 2 * jb)
            wub, wures = ws.get(t * PER + 8 + 2 * jb + 1)
            wgv, wuv = v3(wgb, 16), v3(wub, 16)
            if jb == NJ // 2 - 4:
                wds.ensure(t * 16 + 1)
            for sub in range(2):
                j = jb * 2 + sub
                pg, pgr = cx.bank()
                mm_group(Sc, pg, [(wgv[:, c, sub * 128:(sub + 1) * 128], hT[:, c, :]) for c in range(16)],
                         [wgres, hT_res], pgr)
                pu, pur = cx.bank()
                mm_group(Sc, pu, [(wuv[:, c, sub * 128:(sub + 1) * 128], hT[:, c, :]) for c in range(16)],
                         [wures, hT_res], pur)
                sgb, sgr = sg[j % 2]
                Sc.op("act", lambda e, sgb=sgb, pg=pg: e.activation(out=sgb, in_=pg, func=AF.Silu), reads=[pgr],
                      writes=[sgr])
                Sc.op("dve", lambda e, j=j, sgb=sgb, pu=pu: e.tensor_tensor(out=aT[0][:, j, :], in0=sgb, in1=pu,
                                                                            op=ALU.mult),
                      reads=[sgr, pur], writes=[aT[1]])
        for nb in range(16):
            buf, res = wds.get(t * 16 + nb)
            wdv = v3(buf, NJ)
            pb, pr = cx.bank()
            mm_group(Sc, pb, [(wdv[:, j, :], aT[0][:, j, :]) for j in range(NJ)], [res, aT[1]], pr)
            Sc.op("dve", lambda e, nb=nb, pb=pb: e.tensor_tensor(out=x_ap[:, nb, :], in0=x_ap[:, nb, :], in1=pb,
                                                                 op=ALU.add), reads=[pr, x_res], writes=[x_res])
        if not last:
            Sc.dma("sp", tile_view(T.xs, t), x_ap, reads=[x_res], writes=[T.r_xs])
        else:
            rmsnorm_fm(Sc, cx, x_ap, x_res, x_ap, x_res, gf, gf_res, 16, D)
            Sc.dma("sp", tile_view(T.out, t), x_ap, reads=[x_res], writes=[T.r_out])
    Sc.barrier()


INPUT_SPECS = [
    ("x_in", [NT, 128, 16 * 512]),
    ("attn_norm", [DEPTH, 128, 16]),
    ("ffn_norm", [DEPTH, 128, 16]),
    ("final_norm", [128, 16]),
    ("w_in_even", [2, 12, 128, 16 * 512]),
    ("w_in_odd", [2, 6, 128, 16 * 512]),
    ("w_out_even", [2, 8, 128, 16 * 256]),
    ("w_out_odd", [2, 8, 128, 16 * 256]),
    ("w_gate", [DEPTH, NJ // 2, 128, 16 * 256]),
    ("w_up", [DEPTH, NJ // 2, 128, 16 * 256]),
    ("w_down", [DEPTH, 16, 128, NJ * 128]),
    ("c_kv_norm_fm", [2, 128, 2]),
    ("c_kv_norm_bc", [2, 128, 256]),
    ("ident", [128, 128]),
    ("a_rel_T", [2, 8, 257]),
    ("t5_T", [20, 32]),
    ("t5_bc", [128, 640]),
    ("b_lambda_bc", [2, 128, 512]),
    ("b_subln_fm", [2, 128, 2]),
    ("w_ukT", [2, 16, 128, 256]),
    ("w_uv", [2, 16, 128, 2 * 128]),
]


def build(cfg):
    nc = bass.Bass("TRN2", target_bir_lowering=False)
    T = Ctx()
    T.force_x_in = bool(cfg.get("force_x_in", False))
    feed = set(cfg.get("feed", ()))
    dump = set(cfg.get("dump", ()))

    def dram(name, shape, dtype, kind):
        return nc.dram_tensor(name, list(shape), dtype, kind=kind).ap()

    def scratch(name, shape, dtype):
        kind = "Internal"
        if name in feed:
            kind = "ExternalInput"
        elif name in dump:
            kind = "ExternalOutput"
        return dram(name, shape, dtype, kind)

    for (name, shape) in INPUT_SPECS:
        setattr(T, name, dram(name, shape, F32, "ExternalInput"))
    T.out = dram("out", [NT, 128, 16 * 512], F32, "ExternalOutput")
    T.r_x_in, T.r_small, T.r_w, T.r_out = Res(), Res(), Res(), Res()
    T.xs = scratch("xs", [NT, 128, 16 * 512], F32)
    T.qkT_even = scratch("qkT_even", [4096, S], BF16)
    T.v_a = scratch("v_a", [8, 128, 32 * 128], BF16)
    T.v_b = scratch("v_b", [4, 128, 32 * 256], BF16)
    T.mixT = scratch("mixT", [NT, 128, 16 * 512], BF16)
    T.qcT = scratch("qcT", [2048, S], BF16)
    T.clatT = scratch("clatT", [256, S], BF16)
    T.clat_tm = scratch("clat_tm", [128, 32 * 256], BF16)
    T.qiT = scratch("qiT", [512, S], BF16)
    T.kiT = scratch("kiT", [128, S], BF16)
    T.wi_tm = scratch("wi_tm", [128, 32 * 8], F32)
    T.lineA = scratch("lineA", [8, 128 * 2048], F32)
    T.lineT5 = scratch("lineT5", [20, 128 * 2048], F32)
    T.maskT = scratch("maskT", [NT, 128, 32 * 512], BF16)
    for nm in ("xs", "qkT", "v", "mixT", "qcT", "clatT", "clat_tm", "qiT", "kiT", "wi", "lineA", "lineT5", "maskT"):
        setattr(T, "r_" + nm, Res(nm))

    with ExitStack() as stack:
        big = stack.enter_context(nc.sbuf_tensor("big", [128, ARENA_WORDS], F32))
        banks = [stack.enter_context(nc.psum_tensor("bank%d" % i, [128, 512], F32)) for i in range(8)]
        Sc = Sched(nc, stack)
        cx = Ctx()
        cx.arena = Arena(big)
        cx.banks = [(b[:, :], Res("bank%d" % i)) for i, b in enumerate(banks)]
        cx.brr = 0
        cx.evrr = 0
        cx.const_res = Res("const")

        def bank():
            i = cx.brr % 8
            cx.brr += 1
            return cx.banks[i]

        cx.bank = bank

        def alloc_consts():
            A = cx.arena
            cx.ones_f = A.f32(128)
            cx.ones_b = A.bf16(128)
            cx.ident_b = A.bf16(128)
            Sc.op("pool", lambda e: e.memset(cx.ones_f, 1.0), writes=[cx.const_res])
            Sc.op("pool", lambda e: e.memset(cx.ones_b, 1.0), writes=[cx.const_res])
            Sc.dma("pool", cx.ident_b, T.ident, reads=[T.r_small], writes=[cx.const_res])

        cx.alloc_consts = alloc_consts

        for (ph, layer) in cfg["phases"]:
            if ph == "inproj":
                phase_inproj(Sc, cx, layer, T)
            elif ph == "ffn":
                phase_ffn(Sc, cx, layer, T)
            elif ph == "setup_t5":
                phase_setup_t5(Sc, cx, T)
            elif ph == "attnA":
                phase_attnA(Sc, cx, layer, T)
            elif ph == "attnB":
                phase_attnB(Sc, cx, layer, T)
            elif ph == "idx":
                phase_indexer(Sc, cx, layer, T)
            elif ph == "attnC":
                phase_attnC(Sc, cx, layer, T)
            else:
                raise ValueError(ph)
        Sc.finish("sp")

        with nc.Block() as block:
            @block.tensor
            def _(e):
                Sc.replay("pe", e)

            @block.scalar
            def _(e):
                Sc.replay("act", e)

            @block.vector
            def _(e):
                Sc.replay("dve", e)

            @block.gpsimd
            def _(e):
                Sc.replay("pool", e)

            @block.sync
            def _(e):
                Sc.replay("sp", e)
    return nc, Sc


def _blk_cols(W, width, npad=None):
    K, N = W.shape
    if npad is not None and npad > N:
        W = np.concatenate([W, np.zeros((K, npad - N), W.dtype)], axis=1)
        N = npad
    kc = K // 128
    a = W.reshape(kc, 128, N // width, width).transpose(2, 1, 0, 3)
    return np.ascontiguousarray(a).reshape(N // width, 128, kc * width)


def _vec_fm(v):
    n = v.shape[-1] // 128
    return np.ascontiguousarray(np.swapaxes(v.reshape(v.shape[:-1] + (n, 128)), -1, -2))


def _tile_act(xb):
    a = xb.reshape(NT, 512, 16, 128).transpose(0, 3, 2, 1)
    return np.ascontiguousarray(a).reshape(NT, 128, 16 * 512)


def _untile_act(o):
    a = o.reshape(NT, 128, 16, 512).transpose(0, 3, 2, 1)
    return np.ascontiguousarray(a).reshape(S, D)


def prep_shared(inp):
    f = lambda a: np.asarray(a, dtype=np.float32)
    sh = {}
    sh["attn_norm"] = _vec_fm(f(inp["attn_norm"]))
    sh["ffn_norm"] = _vec_fm(f(inp["ffn_norm"]))
    sh["final_norm"] = _vec_fm(f(inp["final_norm"]))
    sh["w_in_even"] = np.stack([_blk_cols(f(inp["even_w_in"][i]), 512) for i in range(2)])
    sh["w_in_odd"] = np.stack([_blk_cols(f(inp["odd_w_in"][i]), 512, 3072) for i in range(2)])
    sh["w_out_even"] = np.stack([_blk_cols(f(inp["even_w_out"][i]), 256) for i in range(2)])
    sh["w_out_odd"] = np.stack([_blk_cols(f(inp["odd_w_out"][i]), 256) for i in range(2)])
    sh["w_gate"] = np.stack([_blk_cols(f(inp["w_gate"][i]), 256) for i in range(DEPTH)])
    sh["w_up"] = np.stack([_blk_cols(f(inp["w_up"][i]), 256) for i in range(DEPTH)])
    sh["w_down"] = np.stack([_blk_cols(f(inp["w_down"][i]), 128) for i in range(DEPTH)])
    ck = f(inp["c_kv_norm"])
    sh["c_kv_norm_fm"] = _vec_fm(ck)
    sh["c_kv_norm_bc"] = np.ascontiguousarray(np.broadcast_to(ck[:, None, :], (2, 128, 256)))
    sh["ident"] = np.eye(128, dtype=np.float32)
    sh["a_rel_T"] = np.ascontiguousarray(f(inp["a_rel_bias"]).transpose(0, 2, 1))
    t5 = f(inp["t5_table"])
    sh["t5_T"] = np.ascontiguousarray(t5.T)
    sh["t5_bc"] = np.ascontiguousarray(np.broadcast_to(t5.reshape(1, 640), (128, 640)))
    bl = f(inp["b_lambda"]).reshape(2, 1, 512)
    sh["b_lambda_bc"] = np.ascontiguousarray(np.broadcast_to(bl, (2, 128, 512)))
    sh["b_subln_fm"] = _vec_fm(f(inp["b_subln"]))
    sh["w_ukT"] = np.ascontiguousarray(f(inp["c_w_uk"]).transpose(0, 1, 3, 2))
    wuv = f(inp["c_w_uv"]).reshape(2, 16, 2, 128, 128).transpose(0, 1, 3, 2, 4)
    sh["w_uv"] = np.ascontiguousarray(wuv).reshape(2, 16, 128, 256)
    return sh


def full_phases():
    ph = [("setup_t5", 0)]
    for layer in range(DEPTH):
        ph.append(("inproj", layer))
        if layer % 2 == 0:
            ph += [("attnA", layer), ("attnB", layer)]
        else:
            ph += [("idx", layer), ("attnC", layer)]
        ph.append(("ffn", layer))
    return ph


def kernel(**inputs):
    sh = prep_shared(inputs)
    x = np.asarray(inputs["x"], dtype=np.float32)
    nc, _ = build({"phases": full_phases()})
    in_maps = []
    for b in range(8):
        m = dict(sh)
        m["x_in"] = _tile_act(x[b])
        in_maps.append(m)
    res = run_bass_kernel_spmd(nc, in_maps, core_ids=list(range(8)))
    out = np.stack([_untile_act(np.asarray(r["out"], dtype=np.float32)) for r in res.results])
    return out
```

```python
import math
from contextlib import ExitStack

import numpy as np
import concourse.bass as bass
import concourse.mybir as mybir
from concourse.bass_utils import run_bass_kernel_spmd

F32 = mybir.dt.float32
BF16 = mybir.dt.bfloat16
AF = mybir.ActivationFunctionType
ALU = mybir.AluOpType
AX = mybir.AxisListType

D = 2048
S = 4096
DEPTH = 4
NT = S // 512
DFF = 5632
NJ = DFF // 128
EPS = 1e-6
NEG = -30000.0
SCALE = 128 ** -0.5
ODD_PROJ = 2888
ARENA_WORDS = 51200
FFN_NBUF = 4


class Res:
    __slots__ = ("name", "w", "r")

    def __init__(self, name=""):
        self.name = name
        self.w = None
        self.r = {}


class Sched:
    ENGS = ("pe", "act", "dve", "pool", "sp")
    NDMA = 8

    def __init__(self, nc, stack):
        self.nc = nc
        self.q = {e: [] for e in self.ENGS}
        self.sem = {e: stack.enter_context(nc.semaphore("s_" + e)) for e in self.ENGS}
        self.cnt = {e: 0 for e in self.ENGS}
        self.known = {e: {} for e in self.ENGS}
        self.pending = {e: {} for e in self.ENGS}
        self.dsem = {}
        self.dcnt = {}
        self.drr = {}
        for qn in ("sp", "pool", "act"):
            nd = self.NDMA
            self.dsem[qn] = [stack.enter_context(nc.semaphore("d_%s%d" % (qn, i))) for i in range(nd)]
            self.drr[qn] = 0
            for s in self.dsem[qn]:
                self.dcnt[id(s)] = 0
        self.nops = 0

    def _collect(self, eng, reads, writes, acc):
        waits = dict(self.pending[eng])
        self.pending[eng] = {}
        kn = self.known[eng]
        own = self.sem[eng] if eng in self.sem else None

        def need(ev):
            if ev is None:
                return
            s, v = ev
            k = id(s)
            if kn.get(k, 0) >= v:
                return
            if k not in waits or waits[k][1] < v:
                waits[k] = (s, v)

        for r in reads:
            need(r.w)
        for w in writes:
            if not (acc and w.w is not None and w.w[0] is own):
                need(w.w)
            for ev in w.r.values():
                need(ev)
        return waits

    def _commit(self, eng, reads, writes, ev, waits):
        kn = self.known[eng]
        for k, (s, v) in waits.items():
            if kn.get(k, 0) < v:
                kn[k] = v
        k = id(ev[0])
        for r in reads:
            r.r[k] = ev
        for w in writes:
            w.w = ev
            w.r = {}

    def op(self, eng, fn, reads=(), writes=(), acc=False):
        waits = self._collect(eng, reads, writes, acc)
        self.cnt[eng] += 1
        ev = (self.sem[eng], self.cnt[eng])
        self._commit(eng, reads, writes, ev, waits)
        self.q[eng].append((list(waits.values()), fn, (self.sem[eng], 1)))
        self.nops += 1
        return ev

    def dma(self, qn, out_ap, in_ap, reads=(), writes=()):
        eng = qn
        i = self.drr[qn] % len(self.dsem[qn])
        self.drr[qn] += 1
        s = self.dsem[qn][i]
        waits = self._collect(eng, reads, writes, False)
        prev = self.dcnt[id(s)]
        if prev > 0 and self.known[eng].get(id(s), 0) < prev:
            waits[id(s)] = (s, prev)
        self.dcnt[id(s)] = prev + 16
        ev = (s, prev + 16)
        self._commit(eng, reads, writes, ev, waits)

        def fn(e, out_ap=out_ap, in_ap=in_ap):
            return e.dma_start(out=out_ap, in_=in_ap)

        self.q[eng].append((list(waits.values()), fn, (s, 16)))
        self.nops += 1
        return ev

    def barrier(self):
        evs = []
        for e in self.ENGS:
            if self.cnt[e] > 0:
                evs.append((self.sem[e], self.cnt[e]))
        for qn in self.dsem:
            for s in self.dsem[qn]:
                if self.dcnt[id(s)] > 0:
                    evs.append((s, self.dcnt[id(s)]))
        for e in self.ENGS:
            kn = self.known[e]
            for (s, v) in evs:
                if kn.get(id(s), 0) < v:
                    p = self.pending[e]
                    if id(s) not in p or p[id(s)][1] < v:
                        p[id(s)] = (s, v)

    def finish(self, eng="sp"):
        self.barrier()
        waits = self.pending[eng]
        self.pending[eng] = {}
        self.q[eng].append((list(waits.values()), None, None))

    def replay(self, eng, e):
        for (waits, fn, inc) in self.q[eng]:
            for (s, v) in waits:
                e.wait_ge(s, v)
            if fn is not None:
                ins = fn(e)
                ins.then_inc(inc[0], inc[1])


class Arena:
    def __init__(self, big):
        self.big = big
        self.off = 0

    def reset(self):
        self.off = 0

    def f32(self, n):
        assert self.off + n <= ARENA_WORDS, "arena overflow %d" % (self.off + n)
        ap = self.big[:, self.off:self.off + n]
        self.off += n
        return ap

    def bf16(self, n):
        w = (n + 1) // 2
        ap = self.f32(w).bitcast(BF16)
        return ap[:, 0:n]


class Ctx:
    pass


def v3(ap, c):
    return ap.rearrange("p (c t) -> p c t", c=c)


def mm_group(Sc, out_ap, terms, reads, out_res):
    terms = list(terms)

    def fn(e):
        n = len(terms)
        ins = None
        for i, (l, r) in enumerate(terms):
            ins = e.matmul(out_ap, l, r, start=(i == 0), stop=(i == n - 1))
        return ins

    return Sc.op("pe", fn, reads=reads, writes=[out_res])


def rmsnorm_fm(Sc, cx, xt, xt_res, hT, hT_res, g, g_res, nchunk, dim, out_f32=None):
    pb, pr = cx.bank()
    for c in range(nchunk):
        sq, sqr = cx.sq[c % len(cx.sq)]
        Sc.op("act", lambda e, o=sq, i=xt[:, c, :]: e.activation(out=o, in_=i, func=AF.Square),
              reads=[xt_res], writes=[sqr])
        Sc.op("pe", lambda e, o=pb, r=sq, c=c: e.matmul(o, cx.ones_b[:, :], r, start=(c == 0), stop=(c == nchunk - 1)),
              reads=[sqr, cx.const_res], writes=[pr], acc=(c > 0))
    rstd, rr = cx.rstd
    Sc.op("act", lambda e: e.activation(out=rstd, in_=pb, func=AF.Sqrt, bias=EPS, scale=1.0 / dim),
          reads=[pr], writes=[rr])
    Sc.op("dve", lambda e: e.reciprocal(out=rstd, in_=rstd), reads=[rr], writes=[rr])
    for c in range(nchunk):
        Sc.op("dve", lambda e, c=c: e.scalar_tensor_tensor(out=hT[:, c, :], in0=xt[:, c, :], scalar=g[:, c:c + 1],
                                                            in1=rstd, op0=ALU.mult, op1=ALU.mult),
              reads=[xt_res, g_res, rr], writes=[hT_res])


class WStream:
    def __init__(self, Sc, bufs, blocks, r_w, pf, hold=1):
        self.Sc, self.bufs, self.blocks, self.r_w, self.pf = Sc, bufs, blocks, r_w, pf
        self.next = 0
        assert pf <= len(bufs) - hold

    def ensure(self, k):
        while self.next < len(self.blocks) and self.next <= k:
            buf, res = self.bufs[self.next % len(self.bufs)]
            blk = self.blocks[self.next]
            n = blk.shape[1]
            self.Sc.dma("pool", buf[:, 0:n], blk, reads=[self.r_w], writes=[res])
            self.next += 1

    def get(self, i):
        self.ensure(i + self.pf)
        return self.bufs[i % len(self.bufs)]


def evac(Sc, cx, out_ap, in_ap, reads, writes, scale=None):
    cx.evrr += 1
    if cx.evrr % 2 == 0:
        if scale is None:
            Sc.op("act", lambda e: e.activation(out=out_ap, in_=in_ap, func=AF.Copy), reads=reads, writes=writes)
        else:
            Sc.op("act", lambda e: e.activation(out=out_ap, in_=in_ap, func=AF.Copy, scale=float(scale)),
                  reads=reads, writes=writes)
    else:
        if scale is None:
            Sc.op("dve", lambda e: e.tensor_copy(out=out_ap, in_=in_ap), reads=reads, writes=writes)
        else:
            Sc.op("dve", lambda e: e.tensor_scalar(out=out_ap, in0=in_ap, scalar1=float(scale), scalar2=None,
                                                   op0=ALU.mult), reads=reads, writes=writes)


T5_EDGES = [1, 2, 3, 4, 5, 6, 7, 8, 12, 16, 23, 32, 46, 64, 91]


def t5_bucket_static(rel):
    n = abs(rel)
    b = sum(1 for e in T5_EDGES if e <= n)
    return b + (16 if rel > 0 else 0)


def tile_view(t_ap, t):
    return v3(t_ap[t], 16)


def phase_inproj(Sc, cx, layer, T):
    even = (layer % 2 == 0)
    A = cx.arena
    A.reset()
    cx.alloc_consts()
    xt = [(v3(A.f32(16 * 512), 16), Res("xt")) for _ in range(2)]
    hTs = [(v3(A.bf16(16 * 512), 16), Res("hT")) for _ in range(2)]
    cx.sq = [(A.bf16(512), Res("sq")) for _ in range(4)]
    cx.rstd = (A.f32(512), Res("rstd"))
    wbufs = [(A.bf16(16 * 512), Res("wb")) for _ in range(3)]
    stage = [(v3(A.bf16(4 * 512), 4), Res("stg")) for _ in range(3)]
    srr = [0]

    def get_stage():
        s_ = stage[srr[0] % 3]
        srr[0] += 1
        return s_

    g = A.f32(16)
    g_res = Res("g")
    Sc.dma("sp", g, T.attn_norm[layer], reads=[T.r_small], writes=[g_res])
    if not even:
        o = layer // 2
        gk = A.f32(2)
        gk_res = Res("gk")
        Sc.dma("sp", gk, T.c_kv_norm_fm[o], reads=[T.r_small], writes=[gk_res])
        gkb = A.f32(256)
        gkb_res = Res("gkb")
        Sc.dma("sp", gkb, T.c_kv_norm_bc[o], reads=[T.r_small], writes=[gkb_res])
        clf = (v3(A.f32(2 * 512), 2), Res("clf"))
        clo = (v3(A.bf16(2 * 512), 2), Res("clo"))
        cltm = (A.f32(256), Res("cltm"))
        cltm_o = (v3(A.bf16(4 * 256), 4), Res("cltmo"))
        ssq = (A.f32(4), Res("ssq"))
        junk = (A.f32(256), Res("junk"))
        wstage = (v3(A.f32(4 * 8), 4), Res("wst"))
    first = (layer == 0 or T.force_x_in)
    xsrc = T.x_in if first else T.xs
    xsrc_res = T.r_x_in if first else T.r_xs
    W = T.w_in_even[layer // 2] if even else T.w_in_odd[layer // 2]
    nblk = 12 if even else 6
    ws = WStream(Sc, wbufs, [W[nb] for t in range(NT) for nb in range(nblk)], T.r_w, 2)

    def load_x(t):
        Sc.dma("sp", xt[t % 2][0], tile_view(xsrc, t), reads=[xsrc_res], writes=[xt[t % 2][1]])

    load_x(0)
    ws.ensure(1)
    for t in range(NT):
        if t + 1 < NT:
            load_x(t + 1)
        x_ap, x_res = xt[t % 2]
        hT, hT_res = hTs[t % 2]
        rmsnorm_fm(Sc, cx, x_ap, x_res, hT, hT_res, g, g_res, 16, D)
        tok = slice(t * 512, (t + 1) * 512)
        for nb in range(nblk):
            wb, wres = ws.get(t * nblk + nb)
            wv = v3(wb, 16)
            if even:
                kind = "tm" if nb in (4, 5, 10, 11) else "fm"
            else:
                kind = "fm" if nb < 4 else ("odd4" if nb == 4 else "odd5")
            if kind == "fm":
                st, st_res = get_stage()
                scale = SCALE if (even and nb in (0, 1, 6, 7)) else None
                for sub in range(4):
                    pb, pr = cx.bank()
                    mm_group(Sc, pb, [(wv[:, c, sub * 128:(sub + 1) * 128], hT[:, c, :]) for c in range(16)],
                             [wres, hT_res], pr)
                    evac(Sc, cx, st[:, sub, :], pb, [pr], [st_res], scale)
                if even:
                    r0 = _even_fm_row(nb)
                    dst = T.qkT_even[r0:r0 + 512, tok]
                    dres = T.r_qkT
                else:
                    dst = T.qcT[nb * 512:(nb + 1) * 512, tok]
                    dres = T.r_qcT
                Sc.dma("pool", dst.rearrange("(s p) t -> p s t", p=128), st, reads=[st_res], writes=[dres])
            elif kind == "tm":
                st, st_res = get_stage()
                for sub in range(4):
                    pb, pr = cx.bank()
                    mm_group(Sc, pb, [(hT[:, c, sub * 128:(sub + 1) * 128], wv[:, c, :]) for c in range(16)],
                             [wres, hT_res], pr)
                    evac(Sc, cx, st[:, sub, :], pb, [pr], [st_res])
                if nb in (4, 5):
                    for hh in range(4):
                        h = (nb - 4) * 4 + hh
                        Sc.dma("pool", v3(T.v_a[h], 32)[:, t * 4:(t + 1) * 4, :], st[:, :, hh * 128:(hh + 1) * 128],
                               reads=[st_res], writes=[T.r_v])
                else:
                    for hh in range(2):
                        h = (nb - 10) * 2 + hh
                        Sc.dma("pool", v3(T.v_b[h], 32)[:, t * 4:(t + 1) * 4, :], st[:, :, hh * 256:(hh + 1) * 256],
                               reads=[st_res], writes=[T.r_v])
            elif kind == "odd4":
                clf_ap, clf_res = clf
                for cc in range(2):
                    pb, pr = cx.bank()
                    mm_group(Sc, pb, [(wv[:, c, cc * 128:(cc + 1) * 128], hT[:, c, :]) for c in range(16)],
                             [wres, hT_res], pr)
                    evac(Sc, cx, clf_ap[:, cc, :], pb, [pr], [clf_res])
                rmsnorm_fm(Sc, cx, clf_ap, clf_res, clo[0], clo[1], gk, gk_res, 2, 256)
                Sc.dma("pool", T.clatT[:, tok].rearrange("(s p) t -> p s t", p=128), clo[0], reads=[clo[1]],
                       writes=[T.r_clatT])
                for sub in range(4):
                    pb, pr = cx.bank()
                    mm_group(Sc, pb[:, 0:256],
                             [(hT[:, c, sub * 128:(sub + 1) * 128], wv[:, c, 0:256]) for c in range(16)],
                             [wres, hT_res], pr)
                    Sc.op("dve", lambda e, pb=pb: e.tensor_copy(out=cltm[0], in_=pb[:, 0:256]), reads=[pr],
                          writes=[cltm[1]])
                    Sc.op("act", lambda e: e.activation(out=junk[0], in_=cltm[0], func=AF.Square,
                                                        accum_out=ssq[0][:, 0:1]),
                          reads=[cltm[1]], writes=[junk[1], ssq[1]])
                    Sc.op("act", lambda e: e.activation(out=ssq[0][:, 1:2], in_=ssq[0][:, 0:1], func=AF.Sqrt, bias=EPS,
                                                        scale=1.0 / 256), reads=[ssq[1]], writes=[ssq[1]])
                    Sc.op("dve", lambda e: e.reciprocal(out=ssq[0][:, 2:3], in_=ssq[0][:, 1:2]), reads=[ssq[1]],
                          writes=[ssq[1]])
                    Sc.op("dve", lambda e, sub=sub: e.scalar_tensor_tensor(
                        out=cltm_o[0][:, sub, :], in0=cltm[0], scalar=ssq[0][:, 2:3], in1=gkb,
                        op0=ALU.mult, op1=ALU.mult), reads=[cltm[1], ssq[1], gkb_res], writes=[cltm_o[1]])
                Sc.dma("pool", v3(T.clat_tm, 32)[:, t * 4:(t + 1) * 4, :], cltm_o[0], reads=[cltm_o[1]],
                       writes=[T.r_clat_tm])
                st, st_res = get_stage()
                for sub in range(2):
                    pb, pr = cx.bank()
                    mm_group(Sc, pb,
                             [(wv[:, c, 256 + sub * 128:256 + (sub + 1) * 128], hT[:, c, :]) for c in range(16)],
                             [wres, hT_res], pr)
                    evac(Sc, cx, st[:, sub, :], pb, [pr], [st_res])
                Sc.dma("pool", T.qiT[0:256, tok].rearrange("(s p) t -> p s t", p=128), st[:, 0:2, :], reads=[st_res],
                       writes=[T.r_qiT])
            else:
                st, st_res = get_stage()
                for sub in range(2):
                    pb, pr = cx.bank()
                    mm_group(Sc, pb, [(wv[:, c, sub * 128:(sub + 1) * 128], hT[:, c, :]) for c in range(16)],
                             [wres, hT_res], pr)
                    evac(Sc, cx, st[:, sub, :], pb, [pr], [st_res])
                pb, pr = cx.bank()
                mm_group(Sc, pb[0:64, :], [(wv[:, c, 256:320], hT[:, c, :]) for c in range(16)], [wres, hT_res], pr)
                evac(Sc, cx, st[0:64, 2, :], pb[0:64, :], [pr], [st_res])
                Sc.dma("pool", T.qiT[256:512, tok].rearrange("(s p) t -> p s t", p=128), st[:, 0:2, :], reads=[st_res],
                       writes=[T.r_qiT])
                Sc.dma("pool", T.kiT[0:64, tok], st[0:64, 2, :], reads=[st_res], writes=[T.r_kiT])
                Sc.dma("pool", T.kiT[64:128, tok], st[0:64, 2, :], reads=[st_res], writes=[T.r_kiT])
                for sub in range(4):
                    pb, pr = cx.bank()
                    mm_group(Sc, pb[:, 0:8],
                             [(hT[:, c, sub * 128:(sub + 1) * 128], wv[:, c, 320:328]) for c in range(16)],
                             [wres, hT_res], pr)
                    Sc.op("dve", lambda e, pb=pb, sub=sub: e.tensor_copy(out=wstage[0][:, sub, :], in_=pb[:, 0:8]),
                          reads=[pr], writes=[wstage[1]])
                Sc.dma("pool", v3(T.wi_tm, 32)[:, t * 4:(t + 1) * 4, :], wstage[0], reads=[wstage[1]], writes=[T.r_wi])
    Sc.barrier()


def _even_fm_row(nb):
    return {0: 0, 1: 512, 2: 1024, 3: 1536, 6: 2048, 7: 2560, 8: 3072, 9: 3584}[nb]


def build_line_buffer(Sc, cx, tabT, tab_res, nh, segs, B_dram, B_res, fline):
    f_ap, f_res = fline
    for (a, b, src, rng) in segs:
        if rng:
            Sc.op("dve", lambda e, a=a, b=b, src=src: e.tensor_copy(out=f_ap[0:nh, a:b], in_=tabT[0:nh, src:src + (b - a)]),
                  reads=[tab_res], writes=[f_res])
        else:
            Sc.op("dve", lambda e, a=a, b=b, src=src: e.tensor_copy(
                out=f_ap[0:nh, a:b], in_=tabT[0:nh, src:src + 1].to_broadcast([nh, b - a])),
                  reads=[tab_res], writes=[f_res])
    pstep = f_ap.ap[0][0]
    src_ap = bass.AP(f_ap.tensor, f_ap.offset, [[pstep, nh], [0, 128], [1, 2048]])
    Sc.dma("pool", B_dram.rearrange("h (r m) -> h r m", m=2048), src_ap, reads=[f_res], writes=[B_res])


def build_bias_tile(Sc, cx, B_dram, B_res, h, base, dst_ap, dst_res, masked_blocks, tl):
    tl_ap, tl_res = tl
    src = bass.AP(B_dram.tensor, h * 128 * 2048 + base, [[2047, 128], [1, 512]])
    Sc.dma("pool", tl_ap, src, reads=[B_res], writes=[tl_res])
    for (a, b0, b1) in masked_blocks:
        Sc.op("pool", lambda e, a=a, b0=b0, b1=b1: e.memset(tl_ap[a * 64:(a + 1) * 64, b0 * 64:b1 * 64], NEG),
              reads=[], writes=[tl_res])
    Sc.op("act", lambda e: e.activation(out=dst_ap, in_=tl_ap, func=AF.Copy), reads=[tl_res], writes=[dst_res])


def mask_blocks(o, rule):
    out = []
    for a in range(2):
        masked = []
        for b in range(8):
            delta = o // 64 + a - b
            if rule == "A":
                ok = (-8 <= delta <= 0)
            elif rule == "B":
                ok = (delta <= 0)
            else:
                ok = True
            masked.append(not ok)
        b = 0
        while b < 8:
            if masked[b]:
                b1 = b
                while b1 < 8 and masked[b1]:
                    b1 += 1
                out.append((a, b, b1))
                b = b1
            else:
                b += 1
    return out


def attn_score(Sc, cx, s_terms, s_reads, bias_col, bias_res):
    sb, sr = cx.sbanks[cx.srr % 2]
    cx.srr += 1
    mm_group(Sc, sb, s_terms, s_reads, sr)
    E, Er = cx.ebufs[cx.err % len(cx.ebufs)]
    cx.err += 1
    if bias_col is None:
        Sc.op("act", lambda e: e.activation(out=E, in_=sb, func=AF.Exp), reads=[sr], writes=[Er])
    else:
        Sc.op("act", lambda e: e.activation(out=E, in_=sb, func=AF.Exp, bias=bias_col), reads=[sr, bias_res],
              writes=[Er])
    return E, Er


def attn_pv(Sc, cx, EE, pv, first, last):
    E, Er = EE
    for (l, lres, acc_ap, acc_res) in pv:
        Sc.op("pe", lambda e, l=l, acc_ap=acc_ap: e.matmul(acc_ap, l, E, start=first, stop=last),
              reads=[Er, lres, cx.const_res], writes=[acc_res], acc=(not first))


def attn_pipeline(Sc, cx, items):
    n = len(items)
    pend = attn_score(Sc, cx, *items[0][0])
    for i in range(n):
        nxt = attn_score(Sc, cx, *items[i + 1][0]) if i + 1 < n else None
        attn_pv(Sc, cx, pend, items[i][1], i == 0, i == n - 1)
        pend = nxt


def phase_attnA(Sc, cx, layer, T):
    e_ = layer // 2
    A = cx.arena
    A.reset()
    cx.alloc_consts()
    biasA = (v3(A.bf16(64 * 512), 64), Res("biasA"))
    tl = [(A.f32(512), Res("tl")) for _ in range(2)]
    tabT = A.f32(260)
    tab_res = Res("tab")
    fline = (A.f32(2048), Res("fline"))
    QT = [(A.bf16(S), Res("QT")) for _ in range(2)]
    KT = [(A.bf16(S), Res("KT")) for _ in range(2)]
    V = [(v3(A.bf16(32 * 128), 32), Res("V")) for _ in range(2)]
    cx.ebufs = [(A.bf16(512), Res("E")) for _ in range(3)]
    cx.err = 0
    rden = [(A.f32(512), Res("rden")) for _ in range(2)]
    ost = [(A.bf16(512), Res("ost")) for _ in range(2)]
    cx.sbanks = cx.banks[0:2]
    cx.srr = 0
    Sc.dma("sp", tabT[0:8, 0:257], T.a_rel_T[e_], reads=[T.r_small], writes=[tab_res])
    segs = [(0, 384, 0, False), (384, 641, 0, True), (641, 2048, 256, False)]
    build_line_buffer(Sc, cx, tabT, tab_res, 8, segs, T.lineA, T.r_lineA, fline)
    offs = [-512, -384, -256, -128, 0, 128, 256, 384]
    n = 0
    for h in range(8):
        for oi, o in enumerate(offs):
            build_bias_tile(Sc, cx, T.lineA, T.r_lineA, h, 512 - o, biasA[0][:, h * 8 + oi, :], biasA[1],
                            mask_blocks(o, "A"), tl[n % 2])
            n += 1

    def load_head(h):
        Sc.dma("sp", QT[h % 2][0], T.qkT_even[h * 128:(h + 1) * 128, :], reads=[T.r_qkT], writes=[QT[h % 2][1]])
        Sc.dma("sp", KT[h % 2][0], T.qkT_even[1024 + h * 128:1024 + (h + 1) * 128, :], reads=[T.r_qkT],
               writes=[KT[h % 2][1]])
        Sc.dma("sp", V[h % 2][0], v3(T.v_a[h], 32), reads=[T.r_v], writes=[V[h % 2][1]])

    load_head(0)
    it = 0
    for h in range(8):
        if h + 1 < 8:
            load_head(h + 1)
        q_ap, q_res = QT[h % 2]
        k_ap, k_res = KT[h % 2]
        v_ap, v_res = V[h % 2]
        for qt in range(NT):
            kts = [kt for kt in range(4 * qt - 4, 4 * qt + 4) if kt >= 0]
            ob, obr = cx.banks[2 + (it % 2) * 2]
            db, dbr = cx.banks[3 + (it % 2) * 2]
            it += 1
            items = []
            for idx, kt in enumerate(kts):
                oi = kt - (4 * qt - 4)
                s_terms = [(k_ap[:, kt * 128:(kt + 1) * 128], q_ap[:, qt * 512:(qt + 1) * 512]),
                           (cx.ident_b, biasA[0][:, h * 8 + oi, :])]
                pv = [(v_ap[:, kt, :], v_res, ob, obr), (cx.ones_b, cx.const_res, db, dbr)]
                items.append(((s_terms, [k_res, q_res, biasA[1], cx.const_res], None, None), pv))
            attn_pipeline(Sc, cx, items)
            rd, rdr = rden[it % 2]
            os_, osr = ost[it % 2]
            Sc.op("dve", lambda e, rd=rd, db=db: e.reciprocal(out=rd, in_=db), reads=[dbr], writes=[rdr])
            Sc.op("dve", lambda e, os_=os_, ob=ob, rd=rd: e.tensor_tensor(out=os_, in0=ob, in1=rd, op=ALU.mult),
                  reads=[obr, rdr], writes=[osr])
            Sc.dma("pool", tile_view(T.mixT, qt)[:, h, :], os_, reads=[osr], writes=[T.r_mixT])
    Sc.barrier()


def t5_line_segs():
    segs = []
    m = 0
    while m < 2048:
        b = t5_bucket_static(1023 - m)
        m1 = m
        while m1 < 2048 and t5_bucket_static(1023 - m1) == b:
            m1 += 1
        segs.append((m, m1, b, False))
        m = m1
    return segs


def phase_setup_t5(Sc, cx, T):
    A = cx.arena
    A.reset()
    cx.alloc_consts()
    tabT = A.f32(32)
    tab_res = Res("tab")
    fline = (A.f32(2048), Res("fline"))
    Sc.dma("sp", tabT[0:20, 0:32], T.t5_T, reads=[T.r_small], writes=[tab_res])
    build_line_buffer(Sc, cx, tabT, tab_res, 20, t5_line_segs(), T.lineT5, T.r_lineT5, fline)
    Sc.barrier()


def phase_attnB(Sc, cx, layer, T):
    e_ = layer // 2
    lam_init = 0.8 - 0.6 * math.exp(-0.3 * layer)
    A = cx.arena
    A.reset()
    cx.alloc_consts()
    biasB = (v3(A.bf16(20 * 512), 20), Res("biasB"))
    tl = [(A.f32(512), Res("tl")) for _ in range(2)]
    QT = [(v3(A.bf16(2 * S), 2), Res("QT")) for _ in range(2)]
    KT = [(v3(A.bf16(2 * S), 2), Res("KT")) for _ in range(2)]
    V = [(v3(A.bf16(32 * 256), 32), Res("V")) for _ in range(2)]
    cx.ebufs = [(A.bf16(512), Res("E")) for _ in range(3)]
    cx.err = 0
    cx.sbanks = cx.banks[0:2]
    cx.srr = 0
    rden = (A.f32(512), Res("rden"))
    o1 = (v3(A.f32(2 * 512), 2), Res("o1"))
    oo = (v3(A.f32(2 * 512), 2), Res("oo"))
    sqb = (v3(A.bf16(2 * 512), 2), Res("sqb"))
    rstd = (A.f32(512), Res("rstdB"))
    ost = [(v3(A.bf16(2 * 512), 2), Res("ost")) for _ in range(2)]
    t5bc = A.f32(640)
    t5bc_res = Res("t5bc")
    Sc.dma("sp", t5bc, T.t5_bc, reads=[T.r_small], writes=[t5bc_res])
    lamb = A.f32(512)
    lam_res = Res("lam")
    Sc.dma("sp", lamb, T.b_lambda_bc[e_], reads=[T.r_small], writes=[lam_res])
    lsm = A.f32(8)
    junk = A.f32(128)
    junk_res = Res("junk")
    gs = A.f32(2)
    gs_res = Res("gs")
    Sc.dma("sp", gs, T.b_subln_fm[e_], reads=[T.r_small], writes=[gs_res])
    Sc.op("dve", lambda e: e.tensor_scalar(out=gs, in0=gs, scalar1=float(1.0 - lam_init), scalar2=None, op0=ALU.mult),
          reads=[gs_res], writes=[gs_res])
    for i in range(2):
        Sc.op("dve", lambda e, i=i: e.tensor_tensor(out=junk, in0=lamb[:, (2 * i) * 128:(2 * i + 1) * 128],
                                                    in1=lamb[:, (2 * i + 1) * 128:(2 * i + 2) * 128], op=ALU.mult),
              reads=[lam_res], writes=[junk_res])
        Sc.op("dve", lambda e, i=i: e.reduce_sum(out=lsm[:, i:i + 1], in_=junk, axis=AX.X), reads=[junk_res],
              writes=[lam_res])
    Sc.op("act", lambda e: e.activation(out=lsm[:, 2:4], in_=lsm[:, 0:2], func=AF.Exp), reads=[lam_res],
          writes=[lam_res])
    Sc.op("dve", lambda e: e.tensor_tensor(out=lsm[:, 4:5], in0=lsm[:, 3:4], in1=lsm[:, 2:3], op=ALU.subtract),
          reads=[lam_res], writes=[lam_res])
    Sc.op("dve", lambda e: e.tensor_scalar(out=lsm[:, 5:6], in0=lsm[:, 4:5], scalar1=float(-lam_init), scalar2=None,
                                           op0=ALU.add), reads=[lam_res], writes=[lam_res])
    neglam = lsm[:, 5:6]
    offs = [-128, 0, 128, 256, 384]
    n = 0
    for h in range(4):
        for oi, o in enumerate(offs):
            build_bias_tile(Sc, cx, T.lineT5, T.r_lineT5, h, 1023 - o, biasB[0][:, h * 5 + oi, :], biasB[1],
                            mask_blocks(o, "B"), tl[n % 2])
            n += 1

    def load_head(h):
        qv = T.qkT_even[2048 + h * 256:2048 + (h + 1) * 256, :].rearrange("(m p) t -> p m t", p=128)
        kv = T.qkT_even[3072 + h * 256:3072 + (h + 1) * 256, :].rearrange("(m p) t -> p m t", p=128)
        Sc.dma("sp", QT[h % 2][0], qv, reads=[T.r_qkT], writes=[QT[h % 2][1]])
        Sc.dma("sp", KT[h % 2][0], kv, reads=[T.r_qkT], writes=[KT[h % 2][1]])
        Sc.dma("sp", V[h % 2][0], v3(T.v_b[h], 32), reads=[T.r_v], writes=[V[h % 2][1]])

    load_head(0)
    it = 0
    for h in range(4):
        if h + 1 < 4:
            load_head(h + 1)
        q_ap, q_res = QT[h % 2]
        k_ap, k_res = KT[h % 2]
        v_ap, v_res = V[h % 2]
        far_col = t5bc[:, 15 * 20 + h:15 * 20 + h + 1]
        for qt in range(NT):
            for m in range(2):
                kts = list(range(0, 4 * qt + 4))
                o0, o0r = cx.banks[2]
                o1b, o1r = cx.banks[3]
                db, dbr = cx.banks[4]
                items = []
                for idx, kt in enumerate(kts):
                    oi = kt - (4 * qt - 1)
                    s_terms = [(k_ap[:, m, kt * 128:(kt + 1) * 128], q_ap[:, m, qt * 512:(qt + 1) * 512])]
                    s_reads = [k_res, q_res]
                    if oi >= 0:
                        s_terms.append((cx.ident_b, biasB[0][:, h * 5 + oi, :]))
                        s_reads += [biasB[1], cx.const_res]
                        bcol, bres = None, None
                    else:
                        bcol, bres = far_col, t5bc_res
                    pv = [(v_ap[:, kt, 0:128], v_res, o0, o0r), (v_ap[:, kt, 128:256], v_res, o1b, o1r),
                          (cx.ones_b, cx.const_res, db, dbr)]
                    items.append(((s_terms, s_reads, bcol, bres), pv))
                attn_pipeline(Sc, cx, items)
                Sc.op("dve", lambda e, db=db: e.reciprocal(out=rden[0], in_=db), reads=[dbr], writes=[rden[1]])
                if m == 0:
                    Sc.op("dve", lambda e, o0=o0: e.tensor_tensor(out=o1[0][:, 0, :], in0=o0, in1=rden[0], op=ALU.mult),
                          reads=[o0r, rden[1]], writes=[o1[1]])
                    Sc.op("dve", lambda e, o1b=o1b: e.tensor_tensor(out=o1[0][:, 1, :], in0=o1b, in1=rden[0],
                                                                    op=ALU.mult),
                          reads=[o1r, rden[1]], writes=[o1[1]])
                else:
                    for cc, (bk, bkr) in enumerate(((o0, o0r), (o1b, o1r))):
                        Sc.op("dve", lambda e, cc=cc, bk=bk: e.tensor_tensor(out=oo[0][:, cc, :], in0=bk, in1=rden[0],
                                                                             op=ALU.mult),
                              reads=[bkr, rden[1]], writes=[oo[1]])
                        Sc.op("dve", lambda e, cc=cc: e.scalar_tensor_tensor(
                            out=oo[0][:, cc, :], in0=oo[0][:, cc, :], scalar=neglam, in1=o1[0][:, cc, :],
                            op0=ALU.mult, op1=ALU.add), reads=[oo[1], o1[1], lam_res], writes=[oo[1]])
                        Sc.op("act", lambda e, cc=cc: e.activation(out=sqb[0][:, cc, :], in_=oo[0][:, cc, :],
                                                                   func=AF.Square), reads=[oo[1]], writes=[sqb[1]])
                    nb_, nbr = cx.banks[5]
                    mm_group(Sc, nb_, [(cx.ones_b, sqb[0][:, 0, :]), (cx.ones_b, sqb[0][:, 1, :])],
                             [sqb[1], cx.const_res], nbr)
                    Sc.op("act", lambda e, nb_=nb_: e.activation(out=rstd[0], in_=nb_, func=AF.Sqrt, bias=EPS,
                                                                 scale=1.0 / 256), reads=[nbr], writes=[rstd[1]])
                    Sc.op("dve", lambda e: e.reciprocal(out=rstd[0], in_=rstd[0]), reads=[rstd[1]], writes=[rstd[1]])
                    os_, osr = ost[it % 2]
                    it += 1
                    for cc in range(2):
                        Sc.op("dve", lambda e, cc=cc, os_=os_: e.scalar_tensor_tensor(
                            out=os_[:, cc, :], in0=oo[0][:, cc, :], scalar=gs[:, cc:cc + 1], in1=rstd[0],
                            op0=ALU.mult, op1=ALU.mult), reads=[oo[1], gs_res, rstd[1]], writes=[osr])
                    Sc.dma("pool", tile_view(T.mixT, qt)[:, 8 + h * 2:8 + h * 2 + 2, :], os_, reads=[osr],
                           writes=[T.r_mixT])
    Sc.barrier()


BIGNEG = -1.0e30
KNOCK = -2.0e30
TOPK = 256


def phase_indexer(Sc, cx, layer, T):
    A = cx.arena
    A.reset()
    cx.alloc_consts()
    kiT2 = (A.bf16(S), Res("kiT2"))
    Sc.dma("sp", kiT2[0], T.kiT, reads=[T.r_kiT], writes=[kiT2[1]])
    wi = (A.f32(256), Res("wi"))
    Sc.dma("sp", wi[0], T.wi_tm, reads=[T.r_wi], writes=[wi[1]])
    absw = (A.f32(256), Res("absw"))
    sgn = (A.f32(256), Res("sgn"))
    Sc.op("act", lambda e: e.activation(out=absw[0], in_=wi[0], func=AF.Abs), reads=[wi[1]], writes=[absw[1]])
    Sc.op("act", lambda e: e.activation(out=sgn[0], in_=wi[0], func=AF.Sign), reads=[wi[1]], writes=[sgn[1]])
    qi = [(v3(A.bf16(4 * 512), 4), Res("qi")) for _ in range(2)]
    sc = [(A.f32(S), Res("sc")) for _ in range(2)]
    rr = [(A.f32(512), Res("r")) for _ in range(3)]
    mx = (A.f32(8), Res("mx"))
    madd = [(A.bf16(S), Res("madd")) for _ in range(2)]
    mT = [(v3(A.bf16(4 * 128), 4), Res("mT")) for _ in range(3)]
    rri = 0
    mti = 0
    tb = [cx.banks[6], cx.banks[7]]
    tbi = 0

    def load_qi(qt):
        Sc.dma("pool", qi[qt % 2][0], T.qiT[:, qt * 512:(qt + 1) * 512].rearrange("(c p) t -> p c t", p=128),
               reads=[T.r_qiT], writes=[qi[qt % 2][1]])

    load_qi(0)
    for i in range(32):
        qt, qs = i // 4, i % 4
        if qs == 0 and qt + 1 < NT:
            load_qi(qt + 1)
        qi_ap, qi_res = qi[qt % 2]
        sc_ap, sc_res = sc[i % 2]
        nst = qt + 1
        ncol = 512 * nst
        nvalid = 128 * (i + 1)
        for st in range(nst):
            for h in range(8):
                pb, pr = cx.banks[(st * 8 + h) % 6]
                ph = (h % 2) * 64
                Sc.op("pe", lambda e, pb=pb, ph=ph, h=h, st=st, qs=qs, qi_ap=qi_ap: e.matmul(
                    pb, qi_ap[ph:ph + 64, h // 2, qs * 128:(qs + 1) * 128],
                    kiT2[0][ph:ph + 64, st * 512:(st + 1) * 512], start=True, stop=True),
                      reads=[qi_res, kiT2[1]], writes=[pr])
                r_ap, r_res = rr[rri % 3]
                rri += 1
                col = i * 8 + h
                Sc.op("act", lambda e, r_ap=r_ap, pb=pb, col=col: e.activation(
                    out=r_ap, in_=pb, func=AF.Relu, scale=absw[0][:, col:col + 1]), reads=[pr, absw[1]],
                      writes=[r_res])
                dst = sc_ap[:, st * 512:(st + 1) * 512]
                if h == 0:
                    Sc.op("pool", lambda e, dst=dst, r_ap=r_ap, col=col: e.tensor_scalar(
                        out=dst, in0=r_ap, scalar1=sgn[0][:, col:col + 1], scalar2=None, op0=ALU.mult),
                          reads=[r_res, sgn[1]], writes=[sc_res])
                else:
                    Sc.op("pool", lambda e, r_ap=r_ap, col=col: e.tensor_scalar(
                        out=r_ap, in0=r_ap, scalar1=sgn[0][:, col:col + 1], scalar2=None, op0=ALU.mult),
                          reads=[r_res, sgn[1]], writes=[r_res])
                    Sc.op("pool", lambda e, dst=dst, r_ap=r_ap: e.tensor_tensor(out=dst, in0=dst, in1=r_ap, op=ALU.add),
                          reads=[r_res], writes=[sc_res])
        if nvalid < ncol:
            Sc.op("pool", lambda e, sc_ap=sc_ap, nvalid=nvalid, ncol=ncol: e.memset(sc_ap[:, nvalid:ncol], BIGNEG),
                  reads=[], writes=[sc_res])
        Sc.op("pool", lambda e, sc_ap=sc_ap, nvalid=nvalid: e.memset(sc_ap[0:64, nvalid - 64:nvalid], BIGNEG),
              reads=[], writes=[sc_res])
        md_ap, md_res = madd[i % 2]
        if nvalid - 64 > TOPK:
            for rnd in range(TOPK // 8):
                Sc.op("dve", lambda e, sc_ap=sc_ap, nvalid=nvalid: e.max(out=mx[0], in_=sc_ap[:, 0:nvalid]),
                      reads=[sc_res], writes=[mx[1]])
                Sc.op("dve", lambda e, sc_ap=sc_ap, nvalid=nvalid: e.match_replace(
                    out=sc_ap[:, 0:nvalid], in_to_replace=mx[0], in_values=sc_ap[:, 0:nvalid], imm_value=KNOCK),
                      reads=[mx[1]], writes=[sc_res])
            Sc.op("dve", lambda e, md_ap=md_ap, sc_ap=sc_ap, ncol=ncol: e.tensor_scalar(
                out=md_ap[:, 0:ncol], in0=sc_ap[:, 0:ncol], scalar1=-1.5e30, scalar2=NEG, op0=ALU.is_ge, op1=ALU.mult),
                  reads=[sc_res], writes=[md_res])
        else:
            Sc.op("dve", lambda e, md_ap=md_ap, sc_ap=sc_ap, ncol=ncol: e.tensor_scalar(
                out=md_ap[:, 0:ncol], in0=sc_ap[:, 0:ncol], scalar1=-1.0e29, scalar2=NEG, op0=ALU.is_lt, op1=ALU.mult),
                  reads=[sc_res], writes=[md_res])
        for ktg in range(nst):
            bk, bkr = tb[tbi % 2]
            tbi += 1
            bkb = bk.bitcast(BF16)

            def tfn(e, bkb=bkb, md_ap=md_ap, ktg=ktg):
                ins = None
                for j in range(4):
                    kt = ktg * 4 + j
                    ins = e.transpose(bkb[:, j * 128:(j + 1) * 128], md_ap[:, kt * 128:(kt + 1) * 128], cx.ident_b)
                return ins

            Sc.op("pe", tfn, reads=[md_res, cx.const_res], writes=[bkr])
            mt_ap, mt_res = mT[mti % 3]
            mti += 1
            Sc.op("act", lambda e, mt_ap=mt_ap, bkb=bkb: e.activation(
                out=mt_ap, in_=bkb[:, 0:512].rearrange("p (c t) -> p c t", c=4), func=AF.Copy), reads=[bkr],
                  writes=[mt_res])
            dst = v3(T.maskT[qt], 32)[:, ktg * 4:(ktg + 1) * 4, qs * 128:(qs + 1) * 128]
            Sc.dma("pool", dst, mt_ap, reads=[mt_res], writes=[T.r_maskT])
    Sc.barrier()


def phase_attnC(Sc, cx, layer, T):
    o_ = layer // 2
    A = cx.arena
    A.reset()
    cx.alloc_consts()
    biasC = (v3(A.bf16(40 * 512), 40), Res("biasC"))
    tl = [(A.f32(512), Res("tl")) for _ in range(2)]
    clatT = (v3(A.bf16(2 * S), 2), Res("clatT"))
    clat = (v3(A.bf16(32 * 256), 32), Res("clat"))
    Sc.dma("pool", clatT[0], T.clatT.rearrange("(c p) t -> p c t", p=128), reads=[T.r_clatT], writes=[clatT[1]])
    Sc.dma("sp", clat[0], v3(T.clat_tm, 32), reads=[T.r_clat_tm], writes=[clat[1]])
    mk = (v3(A.bf16(32 * 512), 32), Res("mk"))
    qh = (v3(A.bf16(8 * 512), 8), Res("qh"))
    wuk = (v3(A.bf16(8 * 256), 8), Res("wuk"))
    wuv = (A.bf16(8 * 256).rearrange("p (h c d) -> p h c d", h=8, c=2), Res("wuv"))
    cx.ebufs = [(A.bf16(512), Res("E")) for _ in range(3)]
    cx.err = 0
    cx.sbanks = cx.banks[0:2]
    cx.srr = 0
    qlat = [(v3(A.bf16(2 * 512), 2), Res("qlat")) for _ in range(2)]
    olat = [(v3(A.bf16(2 * 512), 2), Res("olat")) for _ in range(2)]
    rden = (A.f32(512), Res("rden"))
    ost = [(A.bf16(512), Res("ost")) for _ in range(2)]
    t5bc = A.f32(640)
    t5bc_res = Res("t5bc")
    Sc.dma("sp", t5bc, T.t5_bc, reads=[T.r_small], writes=[t5bc_res])
    offs = [-128, 0, 128, 256, 384]
    it = 0
    for hg in range(2):
        Sc.dma("pool", wuk[0], T.w_ukT[o_, hg * 8:(hg + 1) * 8].rearrange("h p c -> p h c"), reads=[T.r_w],
               writes=[wuk[1]])
        Sc.dma("pool", wuv[0], T.w_uv[o_, hg * 8:(hg + 1) * 8].rearrange("h p (c d) -> p h c d", c=2),
               reads=[T.r_w], writes=[wuv[1]])
        n = 0
        for hl in range(8):
            for oi, o in enumerate(offs):
                build_bias_tile(Sc, cx, T.lineT5, T.r_lineT5, 4 + hg * 8 + hl, 1023 - o, biasC[0][:, hl * 5 + oi, :],
                                biasC[1], [], tl[n % 2])
                n += 1
        for qt in range(NT):
            nkt = 4 * qt + 4
            Sc.dma("sp", mk[0][:, 0:nkt, :], v3(T.maskT[qt], 32)[:, 0:nkt, :], reads=[T.r_maskT], writes=[mk[1]])
            Sc.dma("pool", qh[0], T.qcT[hg * 1024:(hg + 1) * 1024, qt * 512:(qt + 1) * 512].rearrange(
                "(h p) t -> p h t", p=128), reads=[T.r_qcT], writes=[qh[1]])
            for hl in range(8):
                h = hg * 8 + hl
                ql, qlr = qlat[it % 2]
                ol, olr = olat[it % 2]
                os_, osr = ost[it % 2]
                it += 1
                for cc in range(2):
                    pb, pr = cx.banks[5 + cc]
                    mm_group(Sc, pb, [(wuk[0][:, hl, cc * 128:(cc + 1) * 128], qh[0][:, hl, :])], [wuk[1], qh[1]], pr)
                    evac(Sc, cx, ql[:, cc, :], pb, [pr], [qlr], SCALE)
                far_col = t5bc[:, 15 * 20 + 4 + h:15 * 20 + 4 + h + 1]
                o0, o0r = cx.banks[2]
                o1b, o1r = cx.banks[3]
                db, dbr = cx.banks[4]
                items = []
                for kt in range(nkt):
                    oi = kt - (4 * qt - 1)
                    s_terms = [(clatT[0][:, 0, kt * 128:(kt + 1) * 128], ql[:, 0, :]),
                               (clatT[0][:, 1, kt * 128:(kt + 1) * 128], ql[:, 1, :]),
                               (cx.ident_b, mk[0][:, kt, :])]
                    s_reads = [clatT[1], qlr, mk[1], cx.const_res]
                    if oi >= 0:
                        s_terms.append((cx.ident_b, biasC[0][:, hl * 5 + oi, :]))
                        s_reads.append(biasC[1])
                        bcol, bres = None, None
                    else:
                        bcol, bres = far_col, t5bc_res
                    pv = [(clat[0][:, kt, 0:128], clat[1], o0, o0r), (clat[0][:, kt, 128:256], clat[1], o1b, o1r),
                          (cx.ones_b, cx.const_res, db, dbr)]
                    items.append(((s_terms, s_reads, bcol, bres), pv))
                attn_pipeline(Sc, cx, items)
                Sc.op("dve", lambda e, db=db: e.reciprocal(out=rden[0], in_=db), reads=[dbr], writes=[rden[1]])
                for cc, (bk, bkr) in enumerate(((o0, o0r), (o1b, o1r))):
                    Sc.op("dve", lambda e, cc=cc, bk=bk, ol=ol: e.tensor_tensor(out=ol[:, cc, :], in0=bk, in1=rden[0],
                                                                                op=ALU.mult),
                          reads=[bkr, rden[1]], writes=[olr])
                ub, ubr = cx.banks[7]
                mm_group(Sc, ub, [(wuv[0][:, hl, cc, :], ol[:, cc, :]) for cc in range(2)], [wuv[1], olr], ubr)
                evac(Sc, cx, os_, ub, [ubr], [osr])
                Sc.dma("pool", tile_view(T.mixT, qt)[:, h, :], os_, reads=[osr], writes=[T.r_mixT])
    Sc.barrier()


def phase_ffn(Sc, cx, layer, T):
    even = (layer % 2 == 0)
    last = (layer == DEPTH - 1)
    A = cx.arena
    A.reset()
    cx.alloc_consts()
    xt = (v3(A.f32(16 * 512), 16), Res("xt"))
    mixb = (v3(A.bf16(16 * 512), 16), Res("mix"))
    hTb = (v3(A.bf16(16 * 512), 16), Res("hT"))
    aT = (v3(A.bf16(NJ * 512), NJ), Res("aT"))
    cx.sq = [(A.bf16(512), Res("sq")) for _ in range(4)]
    cx.rstd = (A.f32(512), Res("rstd"))
    wo_buf = [(A.bf16(16 * 256), Res("wb")) for _ in range(FFN_NBUF)]
    wd_buf = [(A.bf16(NJ * 128), Res("wd")) for _ in range(2)]
    sg = [(A.f32(512), Res("sg")) for _ in range(2)]
    g = A.f32(16)
    g_res = Res("g")
    Sc.dma("sp", g, T.ffn_norm[layer], reads=[T.r_small], writes=[g_res])
    if last:
        gf = A.f32(16)
        gf_res = Res("gf")
        Sc.dma("sp", gf, T.final_norm, reads=[T.r_small], writes=[gf_res])
    first = (layer == 0 or T.force_x_in)
    xsrc = T.x_in if first else T.xs
    xsrc_res = T.r_x_in if first else T.r_xs
    Wo = T.w_out_even[layer // 2] if even else T.w_out_odd[layer // 2]
    Wg, Wu, Wd = T.w_gate[layer], T.w_up[layer], T.w_down[layer]
    blocks = []
    for t in range(NT):
        blocks += [Wo[nb] for nb in range(8)]
        for jb in range(NJ // 2):
            blocks += [Wg[jb], Wu[jb]]
    PER = 8 + NJ
    ws = WStream(Sc, wo_buf, blocks, T.r_w, FFN_NBUF - 2, hold=2)
    wds = WStream(Sc, wd_buf, [Wd[nb] for t in range(NT) for nb in range(16)], T.r_w, 1)
    ws.ensure(2)
    for t in range(NT):
        x_ap, x_res = xt
        Sc.dma("sp", x_ap, tile_view(xsrc, t), reads=[xsrc_res], writes=[x_res])
        Sc.dma("sp", mixb[0], tile_view(T.mixT, t), reads=[T.r_mixT], writes=[mixb[1]])
        for nb in range(8):
            wb, wres = ws.get(t * PER + nb)
            wv = v3(wb, 16)
            for sub in range(2):
                n = nb * 2 + sub
                pb, pr = cx.bank()
                mm_group(Sc, pb, [(wv[:, c, sub * 128:(sub + 1) * 128], mixb[0][:, c, :]) for c in range(16)],
                         [wres, mixb[1]], pr)
                Sc.op("dve", lambda e, n=n, pb=pb: e.tensor_tensor(out=x_ap[:, n, :], in0=x_ap[:, n, :], in1=pb,
                                                                   op=ALU.add), reads=[pr, x_res], writes=[x_res])
        hT, hT_res = hTb
        rmsnorm_fm(Sc, cx, x_ap, x_res, hT, hT_res, g, g_res, 16, D)
        for jb in range(NJ // 2):
            wgb, wgres = ws.get(t * PER + 8 + 2 * jb)
            wub, wures = ws.get(t * PER + 8 + 2 * jb + 1)
            wgv, wuv = v3(wgb, 16), v3(wub, 16)
            if jb == NJ // 2 - 4:
                wds.ensure(t * 16 + 1)
            for sub in range(2):
                j = jb * 2 + sub
                pg, pgr = cx.bank()
                mm_group(Sc, pg, [(wgv[:, c, sub * 128:(sub + 1) * 128], hT[:, c, :]) for c in range(16)],
                         [wgres, hT_res], pgr)
                pu, pur = cx.bank()
                mm_group(Sc, pu, [(wuv[:, c, sub * 128:(sub + 1) * 128], hT[:, c, :]) for c in range(16)],
                         [wures, hT_res], pur)
                sgb, sgr = sg[j % 2]
                Sc.op("act", lambda e, sgb=sgb, pg=pg: e.activation(out=sgb, in_=pg, func=AF.Silu), reads=[pgr],
                      writes=[sgr])
                Sc.op("dve", lambda e, j=j, sgb=sgb, pu=pu: e.tensor_tensor(out=aT[0][:, j, :], in0=sgb, in1=pu,
                                                                            op=ALU.mult),
                      reads=[sgr, pur], writes=[aT[1]])
        for nb in range(16):
            buf, res = wds.get(t * 16 + nb)
            wdv = v3(buf, NJ)
            pb, pr = cx.bank()
            mm_group(Sc, pb, [(wdv[:, j, :], aT[0][:, j, :]) for j in range(NJ)], [res, aT[1]], pr)
            Sc.op("dve", lambda e, nb=nb, pb=pb: e.tensor_tensor(out=x_ap[:, nb, :], in0=x_ap[:, nb, :], in1=pb,
                                                                 op=ALU.add), reads=[pr, x_res], writes=[x_res])
        if not last:
            Sc.dma("sp", tile_view(T.xs, t), x_ap, reads=[x_res], writes=[T.r_xs])
        else:
            rmsnorm_fm(Sc, cx, x_ap, x_res, x_ap, x_res, gf, gf_res, 16, D)
            Sc.dma("sp", tile_view(T.out, t), x_ap, reads=[x_res], writes=[T.r_out])
    Sc.barrier()


INPUT_SPECS = [
    ("x_in", [NT, 128, 16 * 512]),
    ("attn_norm", [DEPTH, 128, 16]),
    ("ffn_norm", [DEPTH, 128, 16]),
    ("final_norm", [128, 16]),
    ("w_in_even", [2, 12, 128, 16 * 512]),
    ("w_in_odd", [2, 6, 128, 16 * 512]),
    ("w_out_even", [2, 8, 128, 16 * 256]),
    ("w_out_odd", [2, 8, 128, 16 * 256]),
    ("w_gate", [DEPTH, NJ // 2, 128, 16 * 256]),
    ("w_up", [DEPTH, NJ // 2, 128, 16 * 256]),
    ("w_down", [DEPTH, 16, 128, NJ * 128]),
    ("c_kv_norm_fm", [2, 128, 2]),
    ("c_kv_norm_bc", [2, 128, 256]),
    ("ident", [128, 128]),
    ("a_rel_T", [2, 8, 257]),
    ("t5_T", [20, 32]),
    ("t5_bc", [128, 640]),
    ("b_lambda_bc", [2, 128, 512]),
    ("b_subln_fm", [2, 128, 2]),
    ("w_ukT", [2, 16, 128, 256]),
    ("w_uv", [2, 16, 128, 2 * 128]),
]


def build(cfg):
    nc = bass.Bass("TRN2", target_bir_lowering=False)
    T = Ctx()
    T.force_x_in = bool(cfg.get("force_x_in", False))
    feed = set(cfg.get("feed", ()))
    dump = set(cfg.get("dump", ()))

    def dram(name, shape, dtype, kind):
        return nc.dram_tensor(name, list(shape), dtype, kind=kind).ap()

    def scratch(name, shape, dtype):
        kind = "Internal"
        if name in feed:
            kind = "ExternalInput"
        elif name in dump:
            kind = "ExternalOutput"
        return dram(name, shape, dtype, kind)

    for (name, shape) in INPUT_SPECS:
        setattr(T, name, dram(name, shape, F32, "ExternalInput"))
    T.out = dram("out", [NT, 128, 16 * 512], F32, "ExternalOutput")
    T.r_x_in, T.r_small, T.r_w, T.r_out = Res(), Res(), Res(), Res()
    T.xs = scratch("xs", [NT, 128, 16 * 512], F32)
    T.qkT_even = scratch("qkT_even", [4096, S], BF16)
    T.v_a = scratch("v_a", [8, 128, 32 * 128], BF16)
    T.v_b = scratch("v_b", [4, 128, 32 * 256], BF16)
    T.mixT = scratch("mixT", [NT, 128, 16 * 512], BF16)
    T.qcT = scratch("qcT", [2048, S], BF16)
    T.clatT = scratch("clatT", [256, S], BF16)
    T.clat_tm = scratch("clat_tm", [128, 32 * 256], BF16)
    T.qiT = scratch("qiT", [512, S], BF16)
    T.kiT = scratch("kiT", [128, S], BF16)
    T.wi_tm = scratch("wi_tm", [128, 32 * 8], F32)
    T.lineA = scratch("lineA", [8, 128 * 2048], F32)
    T.lineT5 = scratch("lineT5", [20, 128 * 2048], F32)
    T.maskT = scratch("maskT", [NT, 128, 32 * 512], BF16)
    for nm in ("xs", "qkT", "v", "mixT", "qcT", "clatT", "clat_tm", "qiT", "kiT", "wi", "lineA", "lineT5", "maskT"):
        setattr(T, "r_" + nm, Res(nm))

    with ExitStack() as stack:
        big = stack.enter_context(nc.sbuf_tensor("big", [128, ARENA_WORDS], F32))
        banks = [stack.enter_context(nc.psum_tensor("bank%d" % i, [128, 512], F32)) for i in range(8)]
        Sc = Sched(nc, stack)
        cx = Ctx()
        cx.arena = Arena(big)
        cx.banks = [(b[:, :], Res("bank%d" % i)) for i, b in enumerate(banks)]
        cx.brr = 0
        cx.evrr = 0
        cx.const_res = Res("const")

        def bank():
            i = cx.brr % 8
            cx.brr += 1
            return cx.banks[i]

        cx.bank = bank

        def alloc_consts():
            A = cx.arena
            cx.ones_f = A.f32(128)
            cx.ones_b = A.bf16(128)
            cx.ident_b = A.bf16(128)
            Sc.op("pool", lambda e: e.memset(cx.ones_f, 1.0), writes=[cx.const_res])
            Sc.op("pool", lambda e: e.memset(cx.ones_b, 1.0), writes=[cx.const_res])
            Sc.dma("pool", cx.ident_b, T.ident, reads=[T.r_small], writes=[cx.const_res])

        cx.alloc_consts = alloc_consts

        for (ph, layer) in cfg["phases"]:
            if ph == "inproj":
                phase_inproj(Sc, cx, layer, T)
            elif ph == "ffn":
                phase_ffn(Sc, cx, layer, T)
            elif ph == "setup_t5":
                phase_setup_t5(Sc, cx, T)
            elif ph == "attnA":
                phase_attnA(Sc, cx, layer, T)
            elif ph == "attnB":
                phase_attnB(Sc, cx, layer, T)
            elif ph == "idx":
                phase_indexer(Sc, cx, layer, T)
            elif ph == "attnC":
                phase_attnC(Sc, cx, layer, T)
            else:
                raise ValueError(ph)
        Sc.finish("sp")

        with nc.Block() as block:
            @block.tensor
            def _(e):
                Sc.replay("pe", e)

            @block.scalar
            def _(e):
                Sc.replay("act", e)

            @block.vector
            def _(e):
                Sc.replay("dve", e)

            @block.gpsimd
            def _(e):
                Sc.replay("pool", e)

            @block.sync
            def _(e):
                Sc.replay("sp", e)
    return nc, Sc


def _blk_cols(W, width, npad=None):
    K, N = W.shape
    if npad is not None and npad > N:
        W = np.concatenate([W, np.zeros((K, npad - N), W.dtype)], axis=1)
        N = npad
    kc = K // 128
    a = W.reshape(kc, 128, N // width, width).transpose(2, 1, 0, 3)
    return np.ascontiguousarray(a).reshape(N // width, 128, kc * width)


def _vec_fm(v):
    n = v.shape[-1] // 128
    return np.ascontiguousarray(np.swapaxes(v.reshape(v.shape[:-1] + (n, 128)), -1, -2))


def _tile_act(xb):
    a = xb.reshape(NT, 512, 16, 128).transpose(0, 3, 2, 1)
    return np.ascontiguousarray(a).reshape(NT, 128, 16 * 512)


def _untile_act(o):
    a = o.reshape(NT, 128, 16, 512).transpose(0, 3, 2, 1)
    return np.ascontiguousarray(a).reshape(S, D)


def prep_shared(inp):
    f = lambda a: np.asarray(a, dtype=np.float32)
    sh = {}
    sh["attn_norm"] = _vec_fm(f(inp["attn_norm"]))
    sh["ffn_norm"] = _vec_fm(f(inp["ffn_norm"]))
    sh["final_norm"] = _vec_fm(f(inp["final_norm"]))
    sh["w_in_even"] = np.stack([_blk_cols(f(inp["even_w_in"][i]), 512) for i in range(2)])
    sh["w_in_odd"] = np.stack([_blk_cols(f(inp["odd_w_in"][i]), 512, 3072) for i in range(2)])
    sh["w_out_even"] = np.stack([_blk_cols(f(inp["even_w_out"][i]), 256) for i in range(2)])
    sh["w_out_odd"] = np.stack([_blk_cols(f(inp["odd_w_out"][i]), 256) for i in range(2)])
    sh["w_gate"] = np.stack([_blk_cols(f(inp["w_gate"][i]), 256) for i in range(DEPTH)])
    sh["w_up"] = np.stack([_blk_cols(f(inp["w_up"][i]), 256) for i in range(DEPTH)])
    sh["w_down"] = np.stack([_blk_cols(f(inp["w_down"][i]), 128) for i in range(DEPTH)])
    ck = f(inp["c_kv_norm"])
    sh["c_kv_norm_fm"] = _vec_fm(ck)
    sh["c_kv_norm_bc"] = np.ascontiguousarray(np.broadcast_to(ck[:, None, :], (2, 128, 256)))
    sh["ident"] = np.eye(128, dtype=np.float32)
    sh["a_rel_T"] = np.ascontiguousarray(f(inp["a_rel_bias"]).transpose(0, 2, 1))
    t5 = f(inp["t5_table"])
    sh["t5_T"] = np.ascontiguousarray(t5.T)
    sh["t5_bc"] = np.ascontiguousarray(np.broadcast_to(t5.reshape(1, 640), (128, 640)))
    bl = f(inp["b_lambda"]).reshape(2, 1, 512)
    sh["b_lambda_bc"] = np.ascontiguousarray(np.broadcast_to(bl, (2, 128, 512)))
    sh["b_subln_fm"] = _vec_fm(f(inp["b_subln"]))
    sh["w_ukT"] = np.ascontiguousarray(f(inp["c_w_uk"]).transpose(0, 1, 3, 2))
    wuv = f(inp["c_w_uv"]).reshape(2, 16, 2, 128, 128).transpose(0, 1, 3, 2, 4)
    sh["w_uv"] = np.ascontiguousarray(wuv).reshape(2, 16, 128, 256)
    return sh


def full_phases():
    ph = [("setup_t5", 0)]
    for layer in range(DEPTH):
        ph.append(("inproj", layer))
        if layer % 2 == 0:
            ph += [("attnA", layer), ("attnB", layer)]
        else:
            ph += [("idx", layer), ("attnC", layer)]
        ph.append(("ffn", layer))
    return ph


def kernel(**inputs):
    sh = prep_shared(inputs)
    x = np.asarray(inputs["x"], dtype=np.float32)
    nc, _ = build({"phases": full_phases()})
    in_maps = []
    for b in range(8):
        m = dict(sh)
        m["x_in"] = _tile_act(x[b])
        in_maps.append(m)
    res = run_bass_kernel_spmd(nc, in_maps, core_ids=list(range(8)))
    out = np.stack([_untile_act(np.asarray(r["out"], dtype=np.float32)) for r in res.results])
    return out
```
